# Optimizing a Trainium2 kernel written in Bass

```python
import jax, jax.numpy as jnp
from jax import lax
import numpy as np

D_MODEL = 1024
BATCH = 4
SEQ = 8192
DEPTH = 4

GRID_W = 64
CTX_LEN = 256
EPS = 1e-6
SSD_HEADS = 8
SSD_HEAD_DIM = 64
SSD_INNER = SSD_HEADS * SSD_HEAD_DIM
SSD_GROUPS = 2
SSD_STATE = 64
SSD_CONV = 5
SSD_CHUNK = 128
SSD_XBC = SSD_INNER + 2 * SSD_GROUPS * SSD_STATE
POOL_WINDOWS = (2, 4, 8, 16)
POOL_WIDTH = 512
POOL_GROUP = POOL_WIDTH // len(POOL_WINDOWS)
RET_HEADS = 8
RET_QK_DIM = 64
RET_V_DIM = 64
RET_QK_WIDTH = RET_HEADS * RET_QK_DIM
RET_WIDTH = RET_HEADS * RET_V_DIM
RET_CHUNK = 128
ROPE_BASE = 10000.0
N_BRANCH = 3
BRANCH_WIDTH = 512
COL_Z = 0
COL_XBC = COL_Z + SSD_INNER
COL_DT = COL_XBC + SSD_XBC
COL_POOL = COL_DT + 2 * SSD_HEADS
COL_Q = COL_POOL + POOL_WIDTH
COL_K = COL_Q + RET_QK_WIDTH
COL_V = COL_K + RET_QK_WIDTH
COL_G = COL_V + RET_WIDTH
COL_GATE = COL_G + RET_WIDTH
IN_COLS = COL_GATE + N_BRANCH * D_MODEL
D_FF = 2816
N_EXPERTS = 8
TOP_K = 2
EXPERT_FF = 3584
MOE_BLOCK = 256
N_DENSE = (DEPTH + 1) // 2
N_MOE = DEPTH // 2

kernel_name = 'hybrid_ssd_pool_retention_moe_dit'


def rmsnorm(x, g):
    xf = x.astype(jnp.float32)
    y = xf * lax.rsqrt(jnp.mean(xf * xf, axis=-1, keepdims=True) + EPS)
    return (y * g.astype(jnp.float32)).astype(x.dtype)


def adaln(cvec, w, b):
    m = jax.nn.silu(cvec) @ w + b
    return jnp.split(m[:, None, :], 6, axis=-1)


def modulate(h, shift, scale):
    return h * (1 + scale) + shift


def flip(a):
    return jnp.flip(a, axis=1)


def centred_dwconv(u, w, bias):
    ch = u.shape[-1]
    pad = w.shape[0] // 2
    y = lax.conv_general_dilated(u, w[:, None, :].astype(u.dtype), window_strides=(1,),
                                 padding=[(pad, pad)], dimension_numbers=('NWC', 'WIO', 'NWC'),
                                 feature_group_count=ch)
    return y + bias


def ssd_chunk_scan(xdt, da, bm, cm, s0):
    b, l, h, p = xdt.shape
    n = bm.shape[-1]
    q = SSD_CHUNK
    nc = l // q
    X = xdt.reshape(b, nc, q, h, p)
    Bc = bm.reshape(b, nc, q, h, n)
    Cc = cm.reshape(b, nc, q, h, n)
    A = da.astype(jnp.float32).reshape(b, nc, q, h).transpose(0, 3, 1, 2)
    acs = jnp.cumsum(A, axis=-1)
    causal = jnp.tril(jnp.ones((q, q), dtype=bool))
    seg = acs[..., :, None] - acs[..., None, :]
    Lm = jnp.exp(jnp.where(causal, seg, -jnp.inf))
    scores = jnp.einsum('bclhn,bcshn->bhcls', Cc, Bc) * Lm
    y_diag = jnp.einsum('bhcls,bcshp->bclhp', scores, X)
    decay_states = jnp.exp(acs[..., -1:] - acs)
    states = jnp.einsum('bclhn,bhcl,bclhp->bchpn', Bc, decay_states, X)
    chunk_decay = jnp.exp(acs[..., -1])

    def step(s, inp):
        st, dec = inp
        return dec[:, :, None, None] * s + st, s

    s_fin, prev = lax.scan(step, s0, (states.transpose(1, 0, 2, 3, 4), chunk_decay.transpose(2, 0, 1)))
    prev = prev.transpose(1, 0, 2, 3, 4)
    y_off = jnp.einsum('bclhn,bchpn,bhcl->bclhp', Cc, prev, jnp.exp(acs))
    return (y_diag + y_off).reshape(b, l, h, p), s_fin


def retention_chunk_scan(q, k, v, log_gamma, s0):
    b, l, h, dk = q.shape
    dv = v.shape[-1]
    Q = RET_CHUNK
    nc = l // Q
    qc = q.reshape(b, nc, Q, h, dk)
    kc = k.reshape(b, nc, Q, h, dk)
    vc = v.reshape(b, nc, Q, h, dv)
    idx = jnp.arange(Q, dtype=jnp.float32)
    diff = idx[:, None] - idx[None, :]
    dmat = jnp.where(diff >= 0, jnp.exp(log_gamma[:, None, None] * jnp.maximum(diff, 0.0)), 0.0)
    inner = jnp.einsum('bcihd,bcmhd->bchim', qc, kc) * dmat
    y_in = jnp.einsum('bchim,bcmhe->bcihe', inner, vc)
    k_dec = jnp.exp(log_gamma[:, None] * (Q - 1 - idx)[None, :])
    kv = jnp.einsum('bcmhd,hm,bcmhe->bchde', kc, k_dec, vc)
    chunk_dec = jnp.exp(log_gamma * Q)

    def step(s, kv_c):
        return chunk_dec[None, :, None, None] * s + kv_c, s

    s_fin, prev = lax.scan(step, s0, kv.transpose(1, 0, 2, 3, 4))
    prev = prev.transpose(1, 0, 2, 3, 4)
    q_dec = jnp.exp(log_gamma[:, None] * (idx + 1)[None, :])
    y_x = jnp.einsum('bcihd,bchde,hi->bcihe', qc, prev, q_dec)
    return (y_in + y_x).reshape(b, l, h, dv), s_fin


def axial_rope(x, row, col):
    n_pair = x.shape[-1] // 2
    n_axis = n_pair // 2
    inv = ROPE_BASE ** (-jnp.arange(n_axis, dtype=jnp.float32) / n_axis)
    ang = jnp.concatenate([row[:, None] * inv, col[:, None] * inv], axis=-1)[None, :, None, :]
    cos, sin = jnp.cos(ang), jnp.sin(ang)
    x1, x2 = x[..., :n_pair], x[..., n_pair:]
    return jnp.concatenate([x1 * cos - x2 * sin, x1 * sin + x2 * cos], axis=-1)


def head_groupnorm(y):
    y = y.astype(jnp.float32)
    mu = jnp.mean(y, axis=-1, keepdims=True)
    var = jnp.mean(jnp.square(y - mu), axis=-1, keepdims=True)
    return (y - mu) * lax.rsqrt(var + EPS)


def ssd_branch(pc, pl, conv_w, conv_b, dt_bias, a_log, d_skip, norm_g, need_ctx):
    f32 = jnp.float32
    gn = SSD_GROUPS * SSD_STATE
    rep = SSD_HEADS // SSD_GROUPS

    def prep(p):
        b, l = p.shape[:2]
        z = p[..., COL_Z:COL_Z + SSD_INNER]
        xbc = jax.nn.silu(centred_dwconv(p[..., COL_XBC:COL_XBC + SSD_XBC], conv_w, conv_b))
        xs = xbc[..., :SSD_INNER].reshape(b, l, SSD_HEADS, SSD_HEAD_DIM)
        bm = jnp.repeat(xbc[..., SSD_INNER:SSD_INNER + gn].reshape(b, l, SSD_GROUPS, SSD_STATE), rep, axis=2)
        cm = jnp.repeat(xbc[..., SSD_INNER + gn:].reshape(b, l, SSD_GROUPS, SSD_STATE), rep, axis=2)
        dt_raw = p[..., COL_DT:COL_DT + 2 * SSD_HEADS].astype(f32)
        return z, xs, bm, cm, dt_raw

    def direction(xs, dt_raw, d):
        dt = jax.nn.softplus(dt_raw[..., d * SSD_HEADS:(d + 1) * SSD_HEADS] + dt_bias[d].astype(f32))
        da = dt * (-jnp.exp(a_log[d].astype(f32)))
        return xs * dt[..., None], da

    zc, xc, bc, cc, dtc = prep(pc)
    zl, xl, bl, cl, dtl = prep(pl)
    s0 = jnp.zeros((pc.shape[0], SSD_HEADS, SSD_HEAD_DIM, SSD_STATE), f32)
    u_c, a_c = direction(xc, dtc, 0)
    u_l, a_l = direction(xl, dtl, 0)
    yc_f, sc_f = ssd_chunk_scan(u_c, a_c, bc, cc, s0)
    yl_f, _ = ssd_chunk_scan(u_l, a_l, bl, cl, sc_f)
    u_c, a_c = direction(xc, dtc, 1)
    u_l, a_l = direction(xl, dtl, 1)
    yc_b, sc_b = ssd_chunk_scan(flip(u_c), flip(a_c), flip(bc), flip(cc), s0)
    yl_b, _ = ssd_chunk_scan(flip(u_l), flip(a_l), flip(bl), flip(cl), sc_b)

    def finish(y_f, y_b, xs, z):
        b, l = xs.shape[:2]
        y = y_f + flip(y_b) + d_skip.astype(f32)[:, None] * xs
        return rmsnorm(y.reshape(b, l, SSD_INNER) * jax.nn.silu(z.astype(f32)), norm_g)

    out_l = finish(yl_f, yl_b, xl, zl)
    out_c = finish(yc_f, yc_b, xc, zc) if need_ctx else None
    return out_c, out_l


def centred_window_mean(u, w):
    b, l, ch = u.shape
    left = w // 2
    right = w - 1 - left
    cs = jnp.concatenate([jnp.zeros((b, 1, ch), jnp.float32), jnp.cumsum(u.astype(jnp.float32), axis=1)], axis=1)
    t = jnp.arange(l)
    lo = jnp.maximum(t - left, 0)
    hi = jnp.minimum(t + right, l - 1) + 1
    s = jnp.take(cs, hi, axis=1) - jnp.take(cs, lo, axis=1)
    return s / (hi - lo).astype(jnp.float32)[None, :, None]


def pool_branch(p, pool_w, pool_scale):
    u = p[..., COL_POOL:COL_POOL + POOL_WIDTH]
    outs = []
    for gi, w in enumerate(POOL_WINDOWS):
        ug = u[..., gi * POOL_GROUP:(gi + 1) * POOL_GROUP]
        mixed = centred_window_mean(ug, w) - ug.astype(jnp.float32)
        outs.append(mixed @ pool_w[gi])
    return jnp.concatenate(outs, axis=-1) * pool_scale


def retention_branch(pc, pl, decay_logit, row, col, need_ctx):
    f32 = jnp.float32
    log_gamma = jax.nn.log_sigmoid(decay_logit.astype(f32))

    def split(p):
        b, l = p.shape[:2]
        q = p[..., COL_Q:COL_Q + RET_QK_WIDTH].reshape(b, l, RET_HEADS, RET_QK_DIM)
        k = p[..., COL_K:COL_K + RET_QK_WIDTH].reshape(b, l, RET_HEADS, RET_QK_DIM) * (RET_QK_DIM ** -0.5)
        v = p[..., COL_V:COL_V + RET_WIDTH].reshape(b, l, RET_HEADS, RET_V_DIM)
        g = p[..., COL_G:COL_G + RET_WIDTH]
        return q, k, v, g

    qc, kc, vc, gc = split(pc)
    ql, kl, vl, gl = split(pl)
    ql = axial_rope(ql, row, col)
    kl = axial_rope(kl, row, col)
    s0 = jnp.zeros((pc.shape[0], RET_HEADS, RET_QK_DIM, RET_V_DIM), f32)
    yc_f, sf = retention_chunk_scan(qc, kc, vc, log_gamma[0], s0)
    yl_f, _ = retention_chunk_scan(ql, kl, vl, log_gamma[0], sf)
    yc_b, sb = retention_chunk_scan(flip(qc), flip(kc), flip(vc), log_gamma[1], s0)
    yl_b, _ = retention_chunk_scan(flip(ql), flip(kl), flip(vl), log_gamma[1], sb)

    def finish(y, g):
        b, l = y.shape[:2]
        return head_groupnorm(y).reshape(b, l, RET_WIDTH) * jax.nn.silu(g.astype(f32))

    out_l = finish(yl_f + flip(yl_b), gl)
    out_c = finish(yc_f + flip(yc_b), gc) if need_ctx else None
    return out_c, out_l


def merge_branches(p, branches, w_branch, w_out):
    b, l = p.shape[:2]
    gates = jax.nn.sigmoid(p[..., COL_GATE:COL_GATE + N_BRANCH * D_MODEL].astype(jnp.float32))
    gates = gates.reshape(b, l, N_BRANCH, D_MODEL)
    acc = 0.0
    for i, br in enumerate(branches):
        acc = acc + gates[:, :, i, :] * (br @ w_branch[i]).astype(jnp.float32)
    return acc.astype(p.dtype) @ w_out


def token_mixer(hc, hl, w_in, conv_w, conv_b, dt_bias, a_log, d_skip, ssd_norm_g, pool_w, pool_scale,
                ret_decay_logit, w_branch, w_out, row, col, need_ctx):
    pc = hc @ w_in
    pl = hl @ w_in
    sc, sl = ssd_branch(pc, pl, conv_w, conv_b, dt_bias, a_log, d_skip, ssd_norm_g, need_ctx)
    rc, rl = retention_branch(pc, pl, ret_decay_logit, row, col, need_ctx)
    out_l = merge_branches(pl, (sl, pool_branch(pl, pool_w, pool_scale), rl), w_branch, w_out)
    out_c = merge_branches(pc, (sc, pool_branch(pc, pool_w, pool_scale), rc), w_branch, w_out) if need_ctx else None
    return out_c, out_l


def swiglu(h, w13, w2):
    g, u = jnp.split(h @ w13, 2, axis=-1)
    return (jax.nn.silu(g) * u) @ w2


def moe_swiglu(h, w_router, w13, w2):
    n, d = h.shape
    logits = (h @ w_router).astype(jnp.float32)
    top_v, top_i = lax.top_k(logits, TOP_K)
    top_w = jax.nn.softmax(top_v, axis=-1)
    n_slots = n * TOP_K
    e_flat = top_i.reshape(-1)
    w_flat = top_w.reshape(-1)
    tok_flat = jnp.repeat(jnp.arange(n, dtype=jnp.int32), TOP_K)
    order = jnp.argsort(e_flat)
    e_sorted = e_flat[order]
    tok_sorted = tok_flat[order]
    w_sorted = w_flat[order]
    counts = jnp.bincount(e_flat, length=N_EXPERTS)
    starts = jnp.cumsum(counts) - counts
    padded = (counts + MOE_BLOCK - 1) // MOE_BLOCK * MOE_BLOCK
    pad_ends = jnp.cumsum(padded)
    pad_starts = pad_ends - padded
    dest = pad_starts[e_sorted] + jnp.arange(n_slots) - starts[e_sorted]
    n_blocks = -(-(n_slots + N_EXPERTS * (MOE_BLOCK - 1)) // MOE_BLOCK)
    cap = n_blocks * MOE_BLOCK
    buf_tok = jnp.zeros((cap,), jnp.int32).at[dest].set(tok_sorted)
    buf_w = jnp.zeros((cap,), jnp.float32).at[dest].set(w_sorted)
    block_e = jnp.minimum(jnp.searchsorted(pad_ends, jnp.arange(n_blocks) * MOE_BLOCK, side='right'), N_EXPERTS - 1)
    xb = h[buf_tok].reshape(n_blocks, MOE_BLOCK, d)

    def run_block(args):
        xblk, e = args
        g, u = jnp.split(xblk @ w13[e], 2, axis=-1)
        return (jax.nn.silu(g) * u) @ w2[e]

    yb = lax.map(run_block, (xb, block_e)).reshape(cap, d).astype(jnp.float32)
    y = jnp.zeros((n, d), jnp.float32).at[buf_tok].add(yb * buf_w[:, None])
    return y.astype(h.dtype)


def setup_inputs(seed: int = 0) -> dict:
    key = jax.random.key(seed)
    ks = jax.random.split(key, 32)
    nrm = jax.random.normal
    D = D_MODEL
    dt = jnp.exp(jax.random.uniform(ks[10], (DEPTH, 2, SSD_HEADS)) * (np.log(0.1) - np.log(0.001)) + np.log(0.001))
    gamma_exp = 5.0 + jnp.arange(RET_HEADS, dtype=jnp.float32)
    ret_logit = jnp.log(2.0 ** gamma_exp - 1.0)
    return {
        'x': nrm(ks[0], (BATCH, SEQ, D), jnp.float32),
        'c': nrm(ks[1], (BATCH, D), jnp.float32),
        'ctx': nrm(ks[2], (BATCH, CTX_LEN, D), jnp.float32),
        'c_ctx': nrm(ks[3], (D,), jnp.float32),
        'ada_w': nrm(ks[4], (DEPTH, D, 6 * D), jnp.float32) * (0.5 * D ** -0.5),
        'ada_b': nrm(ks[5], (DEPTH, 6 * D), jnp.float32) * 0.02,
        'norm1_g': 1.0 + 0.02 * nrm(ks[6], (DEPTH, D), jnp.float32),
        'norm2_g': 1.0 + 0.02 * nrm(ks[7], (DEPTH, D), jnp.float32),
        'w_in': nrm(ks[8], (DEPTH, D, IN_COLS), jnp.float32) * D ** -0.5,
        'ssd_conv_w': nrm(ks[9], (DEPTH, SSD_CONV, SSD_XBC), jnp.float32) * SSD_CONV ** -0.5,
        'ssd_conv_b': nrm(ks[11], (DEPTH, SSD_XBC), jnp.float32) * 0.02,
        'ssd_dt_bias': dt + jnp.log(-jnp.expm1(-dt)),
        'ssd_a_log': jnp.log(jax.random.uniform(ks[12], (DEPTH, 2, SSD_HEADS), minval=1.0, maxval=16.0)),
        'ssd_d': 1.0 + 0.02 * nrm(ks[13], (DEPTH, SSD_HEADS), jnp.float32),
        'ssd_norm_g': 1.0 + 0.02 * nrm(ks[14], (DEPTH, SSD_INNER), jnp.float32),
        'pool_w': nrm(ks[15], (DEPTH, len(POOL_WINDOWS), POOL_GROUP, POOL_GROUP), jnp.float32) * POOL_GROUP ** -0.5,
        'pool_scale': 1.0 + 0.02 * nrm(ks[16], (DEPTH, POOL_WIDTH), jnp.float32),
        'ret_decay_logit': ret_logit[None, None, :] + 0.01 * nrm(ks[17], (DEPTH, 2, RET_HEADS), jnp.float32),
        'w_branch': nrm(ks[18], (DEPTH, N_BRANCH, BRANCH_WIDTH, D), jnp.float32) * BRANCH_WIDTH ** -0.5,
        'w_out': nrm(ks[19], (DEPTH, D, D), jnp.float32) * D ** -0.5,
        'ffn_w13': nrm(ks[20], (N_DENSE, D, 2 * D_FF), jnp.float32) * D ** -0.5,
        'ffn_w2': nrm(ks[21], (N_DENSE, D_FF, D), jnp.float32) * D_FF ** -0.5,
        'moe_router': nrm(ks[22], (N_MOE, D, N_EXPERTS), jnp.float32) * D ** -0.5,
        'moe_w13': nrm(ks[23], (N_MOE, N_EXPERTS, D, 2 * EXPERT_FF), jnp.float32) * D ** -0.5,
        'moe_w2': nrm(ks[24], (N_MOE, N_EXPERTS, EXPERT_FF, D), jnp.float32) * EXPERT_FF ** -0.5,
        'final_norm_g': 1.0 + 0.02 * nrm(ks[25], (D,), jnp.float32),
    }


def reference(x, c, ctx, c_ctx, ada_w, ada_b, norm1_g, norm2_g, w_in, ssd_conv_w, ssd_conv_b, ssd_dt_bias,
              ssd_a_log, ssd_d, ssd_norm_g, pool_w, pool_scale, ret_decay_logit, w_branch, w_out,
              ffn_w13, ffn_w2, moe_router, moe_w13, moe_w2, final_norm_g):
    f32 = jnp.float32
    b, seq = x.shape[0], x.shape[1]
    rows = seq // GRID_W
    row = jnp.repeat(jnp.arange(rows, dtype=f32), GRID_W)
    col = (jnp.arange(rows * GRID_W) % GRID_W).astype(f32)
    h_ctx = ctx
    for layer in range(DEPTH):
        need_ctx = layer < DEPTH - 1
        sh1, sc1, g1, sh2, sc2, g2 = adaln(c, ada_w[layer], ada_b[layer])
        csh1, csc1, cg1, csh2, csc2, cg2 = adaln(c_ctx[None, :], ada_w[layer], ada_b[layer])
        hl = modulate(rmsnorm(x, norm1_g[layer]), sh1, sc1)
        hc = modulate(rmsnorm(h_ctx, norm1_g[layer]), csh1, csc1)
        mc, ml = token_mixer(hc, hl, w_in[layer], ssd_conv_w[layer], ssd_conv_b[layer], ssd_dt_bias[layer],
                             ssd_a_log[layer], ssd_d[layer], ssd_norm_g[layer], pool_w[layer], pool_scale[layer],
                             ret_decay_logit[layer], w_branch[layer], w_out[layer], row, col, need_ctx)
        x = x + (g1 * ml).astype(x.dtype)
        if need_ctx:
            h_ctx = h_ctx + (cg1 * mc).astype(h_ctx.dtype)
        tok = modulate(rmsnorm(x, norm2_g[layer]), sh2, sc2).reshape(-1, D_MODEL)
        if need_ctx:
            fc = modulate(rmsnorm(h_ctx, norm2_g[layer]), csh2, csc2)
            tok = jnp.concatenate([fc.reshape(-1, D_MODEL), tok], axis=0)
        if layer % 2 == 0:
            y = swiglu(tok, ffn_w13[layer // 2], ffn_w2[layer // 2])
        else:
            y = moe_swiglu(tok, moe_router[layer // 2], moe_w13[layer // 2], moe_w2[layer // 2])
        n_ctx_tok = tok.shape[0] - b * seq
        x = x + (g2 * y[n_ctx_tok:].reshape(x.shape)).astype(x.dtype)
        if need_ctx:
            h_ctx = h_ctx + (cg2 * y[:n_ctx_tok].reshape(h_ctx.shape)).astype(h_ctx.dtype)
    return rmsnorm(x, final_norm_g)
```

```python
import math
from contextlib import ExitStack
import numpy as np
import ml_dtypes
import concourse.bass as bass
import concourse.mybir as mybir
from concourse.bass_utils import run_bass_kernel_spmd

F32 = mybir.dt.float32
BF16 = mybir.dt.bfloat16
AF = mybir.ActivationFunctionType
ALU = mybir.AluOpType
AX = mybir.AxisListType

D = 1024
SEQ = 8192
CTX = 256
S = SEQ + CTX
NCH = S // 128
DEPTH = 4
EPS = 1e-6
FF = 2816
EFF = 3584
NE = 8
NEXT = 7952
C_Z, C_XBC, C_DT, C_POOL, C_Q, C_K, C_V, C_G, C_GATE, C_QP, C_KP = 0, 512, 1280, 1296, 1808, 2320, 2832, 3344, 3856, 6928, 7440
HPAD = S + 8
NEG = -30000.0


def seg_of(tok0):
    return tok0 + 2 if tok0 < CTX else tok0 + 6


class Buf:
    __slots__ = ("name", "t", "w", "rs", "dsem", "ps")

    def __init__(self, name, t=None):
        self.name = name
        self.t = t
        self.w = None
        self.rs = []
        self.dsem = None
        self.ps = False

    def __getitem__(self, k):
        return self.t[k]


class Sem:
    __slots__ = ("h", "n", "mult")

    def __init__(self, h, mult):
        self.h = h
        self.n = 0
        self.mult = mult


class Eng:
    def __init__(self, fw, e, name):
        self.fw = fw
        self.e = e
        self.name = name
        self.sem = Sem(fw.nc.alloc_semaphore("s_" + name), 1)
        self.seen = {}
        self.selfsync = name in ("act", "dve", "pool")

    def _wait(self, dep, own=False):
        if dep is None:
            return
        s, cnt, g = dep
        if g != self.fw.gen:
            return
        if s is self.sem and not (own and self.selfsync):
            return
        if self.seen.get(id(s), 0) >= cnt:
            return
        self.e.wait_ge(s.h, cnt * s.mult)
        self.seen[id(s)] = cnt

    def deps(self, reads, writes):
        for b in reads:
            self._wait(b.w, own=True)
            if b.ps:
                for r in b.rs:
                    self._wait(r)
        for b in writes:
            self._wait(b.w, own=True)
            for r in b.rs:
                self._wait(r)

    def mark(self, reads, writes, tok):
        for b in reads:
            b.rs.append(tok)
            if len(b.rs) > 8:
                d = {}
                for s, c, g in b.rs:
                    if g != self.fw.gen:
                        continue
                    if id(s) not in d or d[id(s)][1] < c:
                        d[id(s)] = (s, c, g)
                b.rs = list(d.values())
        for b in writes:
            b.w = tok
            b.rs = []

    def op(self, fn, reads=(), writes=()):
        self.deps(reads, writes)
        ins = fn(self.e)
        self.sem.n += 1
        ins.then_inc(self.sem.h, 1)
        self.mark(reads, writes, (self.sem, self.sem.n, self.fw.gen))
        return ins

    def dma(self, out, in_, reads=(), writes=(), **kw):
        self.deps(reads, writes)
        key = None
        for b in list(writes) + list(reads):
            if b.t is not None:
                key = b
                break
        if key is None:
            key = writes[0]
        if key.dsem is None:
            if self.fw.free_dsems:
                key.dsem = self.fw.free_dsems.pop()
            else:
                key.dsem = Sem(self.fw.nc.alloc_semaphore("d_" + key.name), 16)
                self.fw.dsems.append(key.dsem)
            self.fw.live_dsems.append(key.dsem)
        ds = key.dsem
        ins = self.e.dma_start(out=out, in_=in_, **kw)
        ds.n += 1
        ins.then_inc(ds.h, 16)
        self.mark(reads, writes, (ds, ds.n, self.fw.gen))
        return ins


class FW:
    def __init__(self, nc):
        self.nc = nc
        self.dsems = []
        self.free_dsems = []
        self.live_dsems = []
        self.pe = Eng(self, nc.tensor, "pe")
        self.act = Eng(self, nc.scalar, "act")
        self.dve = Eng(self, nc.vector, "dve")
        self.pool = Eng(self, nc.gpsimd, "pool")
        self.sp = Eng(self, nc.sync, "sp")
        self.engs = [self.pe, self.act, self.dve, self.pool, self.sp]
        self.gen = 0
        self.epoch = 0
        self.semE = nc.alloc_semaphore("s_epochE")
        self.semG = nc.alloc_semaphore("s_epochG")

    def barrier(self):
        for e in self.engs:
            for o in self.engs:
                if o is e:
                    if o.sem.n > 0:
                        e.e.wait_ge(o.sem.h, o.sem.n)
                else:
                    e._wait((o.sem, o.sem.n, self.gen))
            for ds in self.dsems:
                e._wait((ds, ds.n, self.gen))
        self.free_dsems.extend(self.live_dsems)
        self.live_dsems = []


def host_consts():
    c = {}
    c["ident"] = np.eye(128, dtype=np.float32)
    k = np.arange(128)
    c["triu"] = (k[:, None] <= k[None, :]).astype(np.float32)
    c["ones"] = np.ones((128, 128), np.float32)
    mf = np.where(k[None, :] >= k[:, None], 0.0, NEG).astype(np.float32)
    mb = np.where(k[:, None] >= k[None, :], 0.0, NEG).astype(np.float32)
    c["maskfb"] = np.stack([np.tile(mf, (1, 4)), np.tile(mb, (1, 4))], axis=1).astype(np.float32)
    e = np.zeros((16, 2, 8, 128), np.float32)
    for d in range(2):
        for h in range(8):
            e[d * 8 + h, d, h, :] = 1.0
    c["eall"] = e
    dp = np.maximum(k[None, :] - k[:, None], 0).astype(np.float32)
    dn = np.maximum(k[:, None] - k[None, :], 0).astype(np.float32)
    mu = (k[None, :] > k[:, None]).astype(np.float32)
    ml = (k[:, None] > k[None, :]).astype(np.float32)
    c["rdiff"] = np.stack([dp, dn, mu, ml, 2.0 * np.eye(128, dtype=np.float32)], axis=1)
    pos = np.stack([127.0 - k, k.astype(np.float64), np.full(128, 128.0)], axis=1).astype(np.float32)
    c["pos"] = pos
    prow = np.stack([np.tile((k + 1.0)[None, :], (128, 1)), np.tile((128.0 - k)[None, :], (128, 1))], axis=1).astype(np.float32)
    c["prow"] = prow
    P = np.zeros((128, 4, 5, 128), np.float32)
    for gi, w in enumerate((2, 4, 8, 16)):
        left = w // 2
        right = w - 1 - left
        for t in range(128):
            for tp in range(t - left, t + right + 1):
                v = 1.0 / w
                if tp < 0:
                    P[tp + 128, gi, 0, t] += v
                elif tp >= 128:
                    P[tp - 128, gi, 4, t] += v
                else:
                    P[tp, gi, 1, t] += v
            P[t, gi, 1, t] -= 1.0
            lo = max(t - left, 0)
            hi = t + right
            cnt = hi - lo + 1
            for tp in range(lo, min(hi, 127) + 1):
                P[tp, gi, 2, t] += 1.0 / cnt
            P[t, gi, 2, t] -= 1.0
            lo = t - left
            hi = min(t + right, 127)
            cnt = hi - lo + 1
            for tp in range(max(lo, 0), hi + 1):
                P[tp, gi, 3, t] += 1.0 / cnt
            P[t, gi, 3, t] -= 1.0
    c["poolP"] = P
    inv = 10000.0 ** (-np.arange(16, dtype=np.float64) / 16)
    t = np.arange(SEQ)
    row = (t // 64).astype(np.float64)
    col = (t % 64).astype(np.float64)
    ang = np.concatenate([row[:, None] * inv[None, :], col[:, None] * inv[None, :]], axis=1)
    cos = np.cos(ang)
    sin = np.sin(ang)
    cos64 = np.concatenate([cos, cos], axis=1)
    sin64 = np.concatenate([-sin, sin], axis=1)
    cosT = np.ones((128, S), np.float32)
    sinT = np.zeros((128, S), np.float32)
    cosT[:, CTX:] = np.tile(cos64.T, (2, 1))
    sinT[:, CTX:] = np.tile(sin64.T, (2, 1))
    c["ropec"] = cosT
    c["ropes"] = sinT
    return c


CONST_SHAPES = None


class K:
    pass


def build(depth=DEPTH, upto=None, dbg=(), cstage=None, upto_layer=0, dstage=None):
    nc = bass.Bass("TRN2", target_bir_lowering=False)
    fw = FW(nc)
    pe, act, dve, pool, sp = fw.pe, fw.act, fw.dve, fw.pool, fw.sp
    cs = host_consts()

    def din(name, shape, dt=F32):
        return nc.dram_tensor(name, list(shape), dt, kind="ExternalInput").ap()

    def dscr(name, shape, dt):
        kind = "ExternalOutput" if name in dbg else "Internal"
        return nc.dram_tensor(name, list(shape), dt, kind=kind).ap()

    xin = din("xin", [S, D])
    cvec = din("cvec", [2, D])
    ada_w = din("ada_w", [DEPTH, D, 6 * D])
    ada_b = din("ada_b", [DEPTH, 6 * D])
    norm1_g = din("norm1_g", [DEPTH, D])
    norm2_g = din("norm2_g", [DEPTH, D])
    w_in = din("w_in", [DEPTH, D, 6928])
    conv_w = din("ssd_conv_w", [DEPTH, 5, 768])
    conv_b = din("ssd_conv_b", [DEPTH, 768])
    dt_bias = din("ssd_dt_bias", [DEPTH, 16])
    a_log = din("ssd_a_log", [DEPTH, 16])
    ssd_d = din("ssd_d", [DEPTH, 8])
    ssd_ng = din("ssd_norm_g", [DEPTH, 512])
    pool_w = din("pool_w", [DEPTH, 4, 128, 128])
    pool_sc = din("pool_scale", [DEPTH, 512])
    ret_dl = din("ret_decay_logit", [DEPTH, 16])
    w_branch = din("w_branch", [DEPTH, 1536, D])
    w_out = din("w_out", [DEPTH, D, D])
    ffn_w13 = din("ffn_w13", [2, D, 2 * FF])
    ffn_w2 = din("ffn_w2", [2, FF, D])
    moe_router = din("moe_router", [2, D, NE])
    nmoe = max(depth // 2, 0)
    moe_w13 = din("moe_w13", [2, NE, D, 2 * EFF] if nmoe else [1, 1, 128, 128])
    moe_w2 = din("moe_w2", [2, NE, EFF, D] if nmoe else [1, 1, 128, 128])
    fin_g = din("final_norm_g", [1, D])
    cin = {k: din("c_" + k, v.shape) for k, v in cs.items()}
    yout = nc.dram_tensor("yout", [SEQ, D], F32, kind="ExternalOutput").ap()

    xres = dscr("xres", [S, D], F32)
    winb = dscr("winb", [DEPTH, D, NEXT], BF16)
    wbrb = dscr("wbrb", [DEPTH, 1536, D], BF16)
    woutb = dscr("woutb", [DEPTH, D, D], BF16)
    f13s = dscr("f13s", [2, FF // 128, 128, 8, 256], BF16)
    f2b = dscr("f2b", [2, FF, D], BF16)
    m13s = dscr("m13s", [2, NE, EFF // 128, 128, 8, 256] if depth // 2 else [1, 1, 1, 128, 8, 256], BF16)
    m2b = dscr("m2b", [2, NE, EFF, D] if depth // 2 else [1, 1, 128, 128], BF16)
    modtab = dscr("modtab", [DEPTH, 2, 6, 128, D], F32)
    hTd = dscr("hTd", [D, HPAD], BF16)
    zs_d = dscr("zs_d", [S, 512], BF16)
    gt_d = dscr("gt_d", [S, 3072], BF16)
    up_d = dscr("up_d", [S, 512], BF16)
    vv_d = dscr("vv_d", [S, 512], BF16)
    gs_d = dscr("gs_d", [S, 512], BF16)
    X_d = dscr("X_d", [S, 512], BF16)
    Btm_d = dscr("Btm_d", [S, 128], BF16)
    BCt_d = dscr("BCt_d", [256, S], BF16)
    dtr_d = dscr("dtr_d", [S, 16], F32)
    Qt_d = dscr("Qt_d", [512, S], BF16)
    Kt_d = dscr("Kt_d", [512, S], BF16)
    Ktm_d = dscr("Ktm_d", [S, 512], BF16)
    Xdt_d = dscr("Xdt_d", [S, 2, 512], BF16)
    cs2_d = dscr("cs2_d", [S, 48], F32)
    cdec_d = dscr("cdec_d", [NCH, 64, 16], F32)
    st_d = dscr("st_d", [NCH, 64, 4, 512], F32)
    prev_d = dscr("prev_d", [NCH, 64, 4, 512], BF16)
    D_x = Buf("dram")

    es_all = ExitStack()

    uniq = [0]

    def SB(es, name, shape, dt):
        uniq[0] += 1
        name = "%s_%d" % (name, uniq[0])
        return Buf(name, es.enter_context(nc.sbuf_tensor(name, list(shape), dt)))

    def PS(es, name, shape, dt=F32):
        uniq[0] += 1
        name = "%s_%d" % (name, uniq[0])
        b = Buf(name, es.enter_context(nc.psum_tensor(name, list(shape), dt)))
        b.ps = True
        return b

    ident_f = SB(es_all, "ident_f", [128, 128], F32)
    ident_b = SB(es_all, "ident_b", [128, 128], BF16)
    sp.dma(ident_f[:], cin["ident"][:, :], writes=[ident_f])
    dve.op(lambda e: e.tensor_copy(out=ident_b[:], in_=ident_f[:]), reads=[ident_f], writes=[ident_b])

    rr = [0]

    def cast_eng():
        rr[0] += 1
        return (dve, pool, act)[rr[0] % 3]

    def do_copy(eng, out, in_, reads, writes):
        if eng is act:
            return eng.op(lambda e: e.copy(out=out, in_=in_), reads=reads, writes=writes)
        return eng.op(lambda e: e.tensor_copy(out=out, in_=in_), reads=reads, writes=writes)

    def phase_cast(layers):
        with ExitStack() as es:
            NB = 3
            CW = 2048
            fbuf = [SB(es, "cf%d" % i, [128, CW], F32) for i in range(NB)]
            bbuf = [SB(es, "cb%d" % i, [128, CW], BF16) for i in range(NB)]
            it = [0]

            def cast2d(src, dst, rows, cols, perm_qk=False):
                for r0 in range(0, rows, 128):
                    for c0 in range(0, cols, CW):
                        cw = min(CW, cols - c0)
                        i = it[0] % NB
                        it[0] += 1
                        sp.dma(fbuf[i][:, 0:cw], src[r0:r0 + 128, c0:c0 + cw], writes=[fbuf[i]])
                        do_copy(cast_eng(), bbuf[i][:, 0:cw], fbuf[i][:, 0:cw], [fbuf[i]], [bbuf[i]])
                        pool.dma(dst[r0:r0 + 128, c0:c0 + cw], bbuf[i][:, 0:cw], reads=[bbuf[i]], writes=[D_x])

            def cast13(src, dst, F_):
                cwh = F_ // 2
                nj = cwh // 128
                for kc in range(8):
                    for q in range(4):
                        i = it[0] % NB
                        it[0] += 1
                        two, j0 = q // 2, (q % 2) * nj
                        sp.dma(fbuf[i][:, 0:cwh], src[kc * 128:(kc + 1) * 128, q * cwh:(q + 1) * cwh], writes=[fbuf[i]])
                        do_copy(cast_eng(), bbuf[i][:, 0:cwh], fbuf[i][:, 0:cwh], [fbuf[i]], [bbuf[i]])
                        for ja in range(0, nj, 7):
                            jb = min(ja + 7, nj)
                            pool.dma(dst[j0 + ja:j0 + jb, :, kc, two * 128:(two + 1) * 128].rearrange("j k i -> k j i"),
                                     bbuf[i][:, ja * 128:jb * 128].rearrange("k (j i) -> k j i", i=128), reads=[bbuf[i]], writes=[D_x])

            for l in layers:
                cast2d(w_in[l], winb[l], D, 6928)
                for r0 in range(0, D, 128):
                    i = it[0] % NB
                    it[0] += 1
                    sp.dma(fbuf[i][:, 0:1024], w_in[l][r0:r0 + 128, C_Q:C_Q + 1024], writes=[fbuf[i]])
                    srcv = fbuf[i][:, 0:1024].rearrange("p (h two j) -> p h two j", two=2, j=32)
                    dstv = bbuf[i][:, 0:1024].rearrange("p (h two j) -> p h two j", two=2, j=32)
                    dve.op(lambda e: e.tensor_copy(out=dstv[:, :, 0, :], in_=srcv[:, :, 1, :]), reads=[fbuf[i]], writes=[bbuf[i]])
                    dve.op(lambda e: e.tensor_copy(out=dstv[:, :, 1, :], in_=srcv[:, :, 0, :]), reads=[fbuf[i]], writes=[bbuf[i]])
                    pool.dma(winb[l][r0:r0 + 128, C_QP:C_QP + 1024], bbuf[i][:, 0:1024], reads=[bbuf[i]], writes=[D_x])
                cast2d(w_branch[l], wbrb[l], 1536, D)
                cast2d(w_out[l], woutb[l], D, D)
                if l % 2 == 0:
                    cast13(ffn_w13[l // 2], f13s[l // 2], FF)
                    cast2d(ffn_w2[l // 2], f2b[l // 2], FF, D)
                elif dstage != 'nomoecast':
                    for e_ in range(NE):
                        cast13(moe_w13[l // 2][e_], m13s[l // 2][e_], EFF)
                        cast2d(moe_w2[l // 2][e_], m2b[l // 2][e_], EFF, D)
        fw.barrier()

    def phase_mod(layers):
        with ExitStack() as es:
            cv = SB(es, "cv", [128, 2, 8], F32)
            scb = SB(es, "scb", [128, 2, 8, 128], F32)
            wbuf = [SB(es, "aw%d" % i, [128, 8, 512], F32) for i in range(2)]
            bb = [SB(es, "ab%d" % i, [128, 512], F32) for i in range(2)]
            ng = SB(es, "ng", [128, 2, D], F32)
            ot = [SB(es, "mo%d" % i, [128, 512], F32) for i in range(2)]
            pm = [PS(es, "pm%d" % i, [128, 512]) for i in range(2)]
            with nc.allow_non_contiguous_dma(reason="tiny"):
                for s_ in range(2):
                    sp.dma(cv[:, s_, :], cvec[s_].rearrange("(kc k) -> k kc", k=128), writes=[cv])
            act.op(lambda e: e.activation(out=cv[:], in_=cv[:], func=AF.Silu), reads=[cv], writes=[cv])
            for s_ in range(2):
                dve.op(lambda e: e.tensor_copy(out=scb[:, s_], in_=cv[:, s_, :].unsqueeze(2).to_broadcast([128, 8, 128])),
                       reads=[cv], writes=[scb])
            n = 0
            for l in layers:
                sp.dma(ng[:, 0, :], norm1_g[l:l + 1, :].partition_broadcast(128), writes=[ng])
                sp.dma(ng[:, 1, :], norm2_g[l:l + 1, :].partition_broadcast(128), writes=[ng])
                for cb in range(12):
                    wi = n % 2
                    n += 1
                    sp.dma(wbuf[wi][:], ada_w[l][:, cb * 512:(cb + 1) * 512].rearrange("(kc k) c -> k kc c", k=128), writes=[wbuf[wi]])
                    sp.dma(bb[wi][:], ada_b[l:l + 1, cb * 512:(cb + 1) * 512].partition_broadcast(128), writes=[bb[wi]])
                    for s_ in range(2):
                        p_ = pm[s_]
                        for kc in range(8):
                            pe.op(lambda e: e.matmul(p_[:], scb[:, s_, kc, :], wbuf[wi][:, kc, :], start=(kc == 0), stop=(kc == 7)),
                                  reads=[scb, wbuf[wi]], writes=[p_])
                        o_ = ot[s_]
                        which = cb // 2
                        half = cb % 2
                        dve.op(lambda e: e.tensor_tensor(out=o_[:], in0=p_[:], in1=bb[wi][:], op=ALU.add), reads=[p_, bb[wi]], writes=[o_])
                        if which in (1, 4):
                            gsel = 0 if which == 1 else 1
                            dve.op(lambda e: e.scalar_tensor_tensor(out=o_[:], in0=o_[:], scalar=1.0, in1=ng[:, gsel, half * 512:(half + 1) * 512],
                                                                    op0=ALU.add, op1=ALU.mult), reads=[o_, ng], writes=[o_])
                        pool.dma(modtab[l, s_, which, :, half * 512:(half + 1) * 512], o_[:], reads=[o_], writes=[D_x])
        fw.barrier()

    def rsqrt(out, in_, scale, bias):
        act.op(lambda e: e.activation(out=out[:], in_=in_[:], func=AF.Sqrt, bias=bias, scale=scale), reads=[in_], writes=[out])
        dve.op(lambda e: e.reciprocal(out=out[:], in_=out[:]), reads=[out], writes=[out])

    def phase_A0(l):
        with ExitStack() as es:
            tabs = SB(es, "tabs", [128, 2, 2, D], F32)
            for s_ in range(2):
                sp.dma(tabs[:, s_, 0, :], modtab[l, s_, 0], writes=[tabs])
                sp.dma(tabs[:, s_, 1, :], modtab[l, s_, 1], writes=[tabs])
            zt = SB(es, "zt", [128, 8, 4], BF16)
            dve.op(lambda e: e.memset(zt[:], 0.0), writes=[zt])
            for kc in range(8):
                for off in (0, 258, 8454):
                    w_ = 2 if off != 258 else 4
                    pool.dma(hTd[kc * 128:(kc + 1) * 128, off:off + w_], zt[:, kc, 0:w_], reads=[zt], writes=[D_x])
            NB = 2
            xt = [SB(es, "xt%d" % i, [128, D], F32) for i in range(3)]
            hb = [SB(es, "hb%d" % i, [128, D], BF16) for i in range(NB)]
            tmp = [SB(es, "tmp%d" % i, [128, D], F32) for i in range(NB)]
            junk = SB(es, "junk", [128, D], F32)
            ss = [SB(es, "ss%d" % i, [128, 1], F32) for i in range(NB)]
            rstd = [SB(es, "rstd%d" % i, [128, 1], F32) for i in range(NB)]
            hT = [SB(es, "hT%d" % i, [128, 8, 512], BF16) for i in range(2)]
            ptr = [PS(es, "ptr%d" % i, [128, 8, 128], BF16) for i in range(2)]
            blocks = [(0, 2)] + [(2 + 4 * j, 4) for j in range(16)]
            first = True
            for bi, (c0, ncb) in enumerate(blocks):
                hTb = hT[bi % 2]
                for ci in range(ncb):
                    c_ = c0 + ci
                    i = c_ % NB
                    x_ = xt[c_ % 3]
                    src = xin if l == 0 else xres
                    sp.dma(x_[:], src[c_ * 128:(c_ + 1) * 128, :], writes=[x_])
                    if l == 0:
                        pool.dma(xres[c_ * 128:(c_ + 1) * 128, :], x_[:], reads=[x_], writes=[D_x])
                    si = 0 if c_ < 2 else 1
                    act.op(lambda e: e.activation(out=junk[:], in_=x_[:], func=AF.Square, accum_out=ss[i][:]), reads=[x_], writes=[junk, ss[i]])
                    rsqrt(rstd[i], ss[i], 1.0 / D, EPS)
                    dve.op(lambda e: e.scalar_tensor_tensor(out=tmp[i][:], in0=x_[:], scalar=rstd[i][:, 0:1], in1=tabs[:, si, 1, :], op0=ALU.mult, op1=ALU.mult),
                           reads=[x_, rstd[i], tabs], writes=[tmp[i]])
                    pool.op(lambda e: e.tensor_tensor(out=hb[i][:], in0=tmp[i][:], in1=tabs[:, si, 0, :], op=ALU.add), reads=[tmp[i], tabs], writes=[hb[i]])
                    p_ = ptr[c_ % 2]
                    for kc in range(8):
                        pe.op(lambda e: e.transpose(p_[:, kc, :], hb[i][:, kc * 128:(kc + 1) * 128], ident_b[:]), reads=[hb[i], ident_b], writes=[p_])
                    do_copy(act if c_ % 2 == 0 else dve, hTb[:, :, ci * 128:(ci + 1) * 128], p_[:], [p_], [hTb])
                n = ncb * 128
                col0 = seg_of(c0 * 128)
                for kc in range(8):
                    pool.dma(hTd[kc * 128:(kc + 1) * 128, col0:col0 + n], hTb[:, kc, 0:n], reads=[hTb], writes=[D_x])
        fw.barrier()


    def phase_A1(l):
        with ExitStack() as es:
            W = SB(es, "W", [128, 8, NEXT], BF16)
            for kc in range(8):
                sp.dma(W[:, kc, :], winb[l][kc * 128:(kc + 1) * 128, :], writes=[W])
            cw = SB(es, "cw", [128, 6, 5], F32)
            cb = SB(es, "cb", [128, 6], F32)
            with nc.allow_non_contiguous_dma(reason="tiny"):
                for pt_ in range(6):
                    sp.dma(cw[:, pt_, :], conv_w[l][:, pt_ * 128:(pt_ + 1) * 128].rearrange("k p -> p k"), writes=[cw])
                sp.dma(cb[:], conv_b[l].rearrange("(t p) -> p t", p=128), writes=[cb])
            hTb = SB(es, "hTb", [128, 8, 516], BF16)
            rc_ = SB(es, "rc_", [128, 512], F32)
            rs_ = SB(es, "rs_", [128, 512], F32)
            xbc = SB(es, "xbc", [128, 6, 512], BF16)
            cv = [SB(es, "cv%d" % i, [128, 512], F32) for i in range(2)]
            qk = SB(es, "qk", [128, 2, 4, 512], BF16)
            t1 = SB(es, "t1", [128, 512], F32)
            t2 = SB(es, "t2", [128, 512], F32)
            outb = SB(es, "outb", [128, 5120], BF16)
            dtt = SB(es, "dtt", [128, 16], F32)
            xtm = SB(es, "xtm", [128, 640], BF16)
            ktm = SB(es, "ktm", [128, 512], BF16)
            pre = PS(es, "pre", [128, 1024])
            pr = [PS(es, "pr%d" % i, [128, 512]) for i in range(2)]
            ptm = [PS(es, "ptm%d" % i, [128, 512]) for i in range(2)]
            ptr = PS(es, "ptrA", [128, 8, 128], BF16)
            blocks = [(0, 256)] + [(256 + 512 * j, 512) for j in range(16)]
            for (tok0, n) in blocks:
                col0 = seg_of(tok0)
                for kc in range(8):
                    sp.dma(hTb[:, kc, 0:n + 4], hTd[kc * 128:(kc + 1) * 128, col0 - 2:col0 + n + 2], writes=[hTb])
                sp.dma(rc_[:, 0:n], cin["ropec"][:, tok0:tok0 + n], writes=[rc_])
                sp.dma(rs_[:, 0:n], cin["ropes"][:, tok0:tok0 + n], writes=[rs_])
                ntt = n // 128
                for pt in range(6):
                    c0 = C_XBC + pt * 128
                    n1 = min(512, n + 4)
                    for kc in range(8):
                        pe.op(lambda e: e.matmul(pre[:, 0:n1], W[:, kc, c0:c0 + 128], hTb[:, kc, 0:n1], start=(kc == 0), stop=(kc == 7)), reads=[W, hTb], writes=[pre])
                    if n + 4 > 512:
                        for kc in range(8):
                            pe.op(lambda e: e.matmul(pre[:, 512:n + 4], W[:, kc, c0:c0 + 128], hTb[:, kc, 512:n + 4], start=(kc == 0), stop=(kc == 7)), reads=[W, hTb], writes=[pre])
                    c_ = cv[pt % 2]
                    dve.op(lambda e: e.tensor_scalar(out=c_[:, 0:n], in0=pre[:, 0:n], scalar1=cw[:, pt, 0:1], scalar2=None, op0=ALU.mult), reads=[pre, cw], writes=[c_])
                    for k in range(1, 5):
                        dve.op(lambda e: e.scalar_tensor_tensor(out=c_[:, 0:n], in0=pre[:, k:k + n], scalar=cw[:, pt, k:k + 1], in1=c_[:, 0:n], op0=ALU.mult, op1=ALU.add),
                               reads=[pre, cw, c_], writes=[c_])
                    act.op(lambda e: e.activation(out=xbc[:, pt, 0:n], in_=c_[:, 0:n], func=AF.Silu, bias=cb[:, pt:pt + 1], scale=1.0), reads=[c_, cb], writes=[xbc])
                pool.dma(BCt_d[0:128, tok0:tok0 + n], xbc[:, 4, 0:n], reads=[xbc], writes=[D_x])
                pool.dma(BCt_d[128:256, tok0:tok0 + n], xbc[:, 5, 0:n], reads=[xbc], writes=[D_x])
                for tt in range(ntt):
                    for pt in range(5):
                        pe.op(lambda e: e.transpose(ptr[:, pt, :], xbc[:, pt, tt * 128:(tt + 1) * 128], ident_b[:]), reads=[xbc, ident_b], writes=[ptr])
                    dve.op(lambda e: e.tensor_copy(out=xtm[:], in_=ptr[:, 0:5, :]), reads=[ptr], writes=[xtm])
                    r0 = tok0 + tt * 128
                    pool.dma(X_d[r0:r0 + 128, :], xtm[:, 0:512], reads=[xtm], writes=[D_x])
                    pool.dma(Btm_d[r0:r0 + 128, :], xtm[:, 512:640], reads=[xtm], writes=[D_x])
                for wh, (cA, cP, dst) in enumerate(((C_Q, C_QP, Qt_d), (C_K, C_KP, Kt_d))):
                    for hp in range(4):
                        for j, cc in enumerate((cA, cP)):
                            for kc in range(8):
                                pe.op(lambda e: e.matmul(pr[j][:, 0:n], W[:, kc, cc + hp * 128:cc + (hp + 1) * 128], hTb[:, kc, 2:2 + n], start=(kc == 0), stop=(kc == 7)),
                                      reads=[W, hTb], writes=[pr[j]])
                        dve.op(lambda e: e.tensor_tensor(out=t1[:, 0:n], in0=pr[0][:, 0:n], in1=rc_[:, 0:n], op=ALU.mult), reads=[pr[0], rc_], writes=[t1])
                        dve.op(lambda e: e.tensor_tensor(out=t2[:, 0:n], in0=pr[1][:, 0:n], in1=rs_[:, 0:n], op=ALU.mult), reads=[pr[1], rs_], writes=[t2])
                        pool.op(lambda e: e.tensor_tensor(out=qk[:, wh, hp, 0:n], in0=t1[:, 0:n], in1=t2[:, 0:n], op=ALU.add), reads=[t1, t2], writes=[qk])
                        pool.dma(dst[hp * 128:(hp + 1) * 128, tok0:tok0 + n], qk[:, wh, hp, 0:n], reads=[qk], writes=[D_x])
                for tt in range(ntt):
                    for hp in range(4):
                        pe.op(lambda e: e.transpose(ptr[:, hp, :], qk[:, 1, hp, tt * 128:(tt + 1) * 128], ident_b[:]), reads=[qk, ident_b], writes=[ptr])
                    dve.op(lambda e: e.tensor_copy(out=ktm[:], in_=ptr[:, 0:4, :]), reads=[ptr], writes=[ktm])
                    r0 = tok0 + tt * 128
                    pool.dma(Ktm_d[r0:r0 + 128, :], ktm[:], reads=[ktm], writes=[D_x])
                for tt in range(ntt):
                    r0 = tok0 + tt * 128
                    lT = lambda kc: hTb[:, kc, 2 + tt * 128:2 + (tt + 1) * 128]
                    specs = [(C_Z, 0, "silu"), (C_POOL, 512, "copy"), (C_V, 1024, "copy"), (C_G, 1536, "silu")] + [(C_GATE + 512 * j, 2048 + 512 * j, "sig") for j in range(6)]
                    for si_, (cc, oc, kind) in enumerate(specs):
                        p_ = ptm[si_ % 2]
                        for kc in range(8):
                            pe.op(lambda e: e.matmul(p_[:], lT(kc), W[:, kc, cc:cc + 512], start=(kc == 0), stop=(kc == 7)), reads=[hTb, W], writes=[p_])
                        if kind == "copy":
                            dve.op(lambda e: e.tensor_copy(out=outb[:, oc:oc + 512], in_=p_[:]), reads=[p_], writes=[outb])
                        else:
                            fn = AF.Silu if kind == "silu" else AF.Sigmoid
                            act.op(lambda e: e.activation(out=outb[:, oc:oc + 512], in_=p_[:], func=fn), reads=[p_], writes=[outb])
                    p_ = ptm[0]
                    for kc in range(8):
                        pe.op(lambda e: e.matmul(p_[:, 0:16], lT(kc), W[:, kc, C_DT:C_DT + 16], start=(kc == 0), stop=(kc == 7)), reads=[hTb, W], writes=[p_])
                    dve.op(lambda e: e.tensor_copy(out=dtt[:], in_=p_[:, 0:16]), reads=[p_], writes=[dtt])
                    pool.dma(zs_d[r0:r0 + 128, :], outb[:, 0:512], reads=[outb], writes=[D_x])
                    pool.dma(up_d[r0:r0 + 128, :], outb[:, 512:1024], reads=[outb], writes=[D_x])
                    pool.dma(vv_d[r0:r0 + 128, :], outb[:, 1024:1536], reads=[outb], writes=[D_x])
                    pool.dma(gs_d[r0:r0 + 128, :], outb[:, 1536:2048], reads=[outb], writes=[D_x])
                    pool.dma(gt_d[r0:r0 + 128, :], outb[:, 2048:5120], reads=[outb], writes=[D_x])
                    pool.dma(dtr_d[r0:r0 + 128, :], dtt[:], reads=[dtt], writes=[D_x])
        fw.barrier()


    brs_d = dscr("brs_d", [S, 1536], BF16)

    def bc_last(ap, n):
        sh = list(ap.shape)
        return ap.unsqueeze(len(sh)).to_broadcast(sh + [n])

    def layer_tabs(es, l):
        T = {}
        raw = SB(es, "raw16", [128, 3, 16], F32)
        sp.dma(raw[:, 0, :], dt_bias[l:l + 1, :].partition_broadcast(128), writes=[raw])
        sp.dma(raw[:, 1, :], a_log[l:l + 1, :].partition_broadcast(128), writes=[raw])
        sp.dma(raw[:, 2, :], ret_dl[l:l + 1, :].partition_broadcast(128), writes=[raw])
        nega = SB(es, "nega", [128, 16], F32)
        act.op(lambda e: e.activation(out=nega[:], in_=raw[:, 1, :], func=AF.Exp), reads=[raw], writes=[nega])
        dve.op(lambda e: e.tensor_scalar(out=nega[:], in0=nega[:], scalar1=-1.0, scalar2=None, op0=ALU.mult), reads=[nega], writes=[nega])
        lg = SB(es, "lg", [128, 16], F32)
        act.op(lambda e: e.activation(out=lg[:], in_=raw[:, 2, :], func=AF.Exp, scale=-1.0), reads=[raw], writes=[lg])
        act.op(lambda e: e.activation(out=lg[:], in_=lg[:], func=AF.Ln, bias=1.0, scale=1.0), reads=[lg], writes=[lg])
        dve.op(lambda e: e.tensor_scalar(out=lg[:], in0=lg[:], scalar1=-1.0, scalar2=None, op0=ALU.mult), reads=[lg], writes=[lg])
        posb = SB(es, "posb", [128, 3], F32)
        sp.dma(posb[:], cin["pos"][:, :], writes=[posb])
        T.update(raw=raw, nega=nega, lg=lg, posb=posb)
        return T

    def phase_B1(l):
        with ExitStack() as es:
            T = layer_tabs(es, l)
            raw, nega, lg, posb = T["raw"], T["nega"], T["lg"], T["posb"]
            kdec = SB(es, "kdec", [128, 2, 8], F32)
            for d in range(2):
                act.op(lambda e: e.activation(out=kdec[:, d, :], in_=lg[:, d * 8:(d + 1) * 8], func=AF.Exp, scale=posb[:, d:d + 1]), reads=[lg, posb], writes=[kdec])
            triu = SB(es, "triu", [128, 128], F32)
            ones = SB(es, "ones", [128, 128], F32)
            sp.dma(triu[:], cin["triu"][:, :], writes=[triu])
            sp.dma(ones[:], cin["ones"][:, :], writes=[ones])
            NB = 2
            Xb = [SB(es, "Xb%d" % i, [128, 512], BF16) for i in range(NB)]
            Bb = [SB(es, "Bb%d" % i, [128, 128], BF16) for i in range(NB)]
            dtr = [SB(es, "dtr%d" % i, [128, 16], F32) for i in range(NB)]
            Kb = [SB(es, "Kb%d" % i, [128, 512], BF16) for i in range(NB)]
            Vb = [SB(es, "Vb%d" % i, [128, 512], BF16) for i in range(NB)]
            dtv = SB(es, "dtv", [128, 16], F32)
            cs2 = [SB(es, "cs2%d" % i, [128, 48], F32) for i in range(NB)]
            t16 = SB(es, "t16", [128, 16], F32)
            dsx = SB(es, "dsx", [128, 16], F32)
            cd = [SB(es, "cd%d" % i, [64, 16], F32) for i in range(NB)]
            Xdt = [SB(es, "Xdt%d" % i, [128, 2, 512], BF16) for i in range(NB)]
            Bdec = SB(es, "Bdec", [128, 2, 8, 64], BF16)
            Vdec = SB(es, "Vdec", [128, 2, 512], BF16)
            stt = [SB(es, "stt%d" % i, [64, 4, 512], F32) for i in range(NB)]
            pc = PS(es, "pc", [128, 512])
            pst = PS(es, "pst", [64, 2, 512])
            pkv = PS(es, "pkv", [64, 2, 512])
            for c in range(NCH):
                i = c % NB
                r0 = c * 128
                sp.dma(Xb[i][:], X_d[r0:r0 + 128, :], writes=[Xb[i]])
                sp.dma(Bb[i][:], Btm_d[r0:r0 + 128, :], writes=[Bb[i]])
                sp.dma(dtr[i][:], dtr_d[r0:r0 + 128, :], writes=[dtr[i]])
                sp.dma(Kb[i][:], Ktm_d[r0:r0 + 128, :], writes=[Kb[i]])
                sp.dma(Vb[i][:], vv_d[r0:r0 + 128, :], writes=[Vb[i]])
                c2 = cs2[i]
                dve.op(lambda e: e.tensor_tensor(out=dtv[:], in0=dtr[i][:], in1=raw[:, 0, :], op=ALU.add), reads=[dtr[i], raw], writes=[dtv])
                act.op(lambda e: e.activation(out=dtv[:], in_=dtv[:], func=AF.Exp), reads=[dtv], writes=[dtv])
                act.op(lambda e: e.activation(out=dtv[:], in_=dtv[:], func=AF.Ln, bias=1.0, scale=1.0), reads=[dtv], writes=[dtv])
                dve.op(lambda e: e.tensor_tensor(out=c2[:, 32:48], in0=dtv[:], in1=nega[:], op=ALU.mult), reads=[dtv, nega], writes=[c2])
                pe.op(lambda e: e.matmul(pc[:, 0:16], triu[:], c2[:, 32:48], start=True, stop=True), reads=[triu, c2], writes=[pc])
                pe.op(lambda e: e.matmul(pc[:, 16:32], ones[:], c2[:, 32:48], start=True, stop=True), reads=[ones, c2], writes=[pc])
                dve.op(lambda e: e.tensor_copy(out=c2[:, 0:32], in_=pc[:, 0:32]), reads=[pc], writes=[c2])
                dve.op(lambda e: e.tensor_tensor(out=c2[:, 8:16], in0=c2[:, 8:16], in1=c2[:, 40:48], op=ALU.subtract), reads=[c2], writes=[c2])
                dve.op(lambda e: e.tensor_tensor(out=t16[:, 0:8], in0=c2[:, 16:24], in1=c2[:, 0:8], op=ALU.subtract), reads=[c2], writes=[t16])
                dve.op(lambda e: e.tensor_copy(out=t16[:, 8:16], in_=c2[:, 8:16]), reads=[c2], writes=[t16])
                act.op(lambda e: e.activation(out=dsx[:], in_=t16[:], func=AF.Exp), reads=[t16], writes=[dsx])
                act.op(lambda e: e.activation(out=cd[i][:], in_=c2[0:64, 16:32], func=AF.Exp), reads=[c2], writes=[cd[i]])
                pool.dma(cdec_d[c], cd[i][:], reads=[cd[i]], writes=[D_x])
                pool.dma(cs2_d[r0:r0 + 128, :], c2[:], reads=[c2], writes=[D_x])
                xd = Xdt[i]
                for d in range(2):
                    dve.op(lambda e: e.tensor_tensor(out=xd[:, d, :].rearrange("p (h q) -> p h q", h=8), in0=Xb[i][:].rearrange("p (h q) -> p h q", h=8),
                                                     in1=bc_last(dtv[:, d * 8:(d + 1) * 8], 64), op=ALU.mult), reads=[Xb[i], dtv], writes=[xd])
                pool.dma(Xdt_d[r0:r0 + 128], xd[:], reads=[xd], writes=[D_x])
                for d in range(2):
                    bview = Bb[i][:].rearrange("p (g n) -> p g n", g=2).unsqueeze(2).to_broadcast([128, 2, 4, 64])
                    dview = bc_last(dsx[:, d * 8:(d + 1) * 8].rearrange("p (g r) -> p g r", g=2), 64)
                    dve.op(lambda e: e.tensor_tensor(out=Bdec[:, d].rearrange("p (g r) n -> p g r n", g=2), in0=bview, in1=dview, op=ALU.mult), reads=[Bb[i], dsx], writes=[Bdec])
                    dve.op(lambda e: e.tensor_tensor(out=Vdec[:, d, :].rearrange("p (h q) -> p h q", h=8), in0=Vb[i][:].rearrange("p (h q) -> p h q", h=8),
                                                     in1=bc_last(kdec[:, d, :], 64), op=ALU.mult), reads=[Vb[i], kdec], writes=[Vdec])
                for d in range(2):
                    for h in range(8):
                        pe.op(lambda e: e.matmul(pst[:, d, h * 64:(h + 1) * 64], Bdec[:, d, h, :], xd[:, d, h * 64:(h + 1) * 64], start=True, stop=True), reads=[Bdec, xd], writes=[pst])
                        pe.op(lambda e: e.matmul(pkv[:, d, h * 64:(h + 1) * 64], Kb[i][:, h * 64:(h + 1) * 64], Vdec[:, d, h * 64:(h + 1) * 64], start=True, stop=True), reads=[Kb[i], Vdec], writes=[pkv])
                st_ = stt[i]
                dve.op(lambda e: e.tensor_copy(out=st_[:, 0:2, :], in_=pst[:]), reads=[pst], writes=[st_])
                act.op(lambda e: e.copy(out=st_[:, 2:4, :], in_=pkv[:]), reads=[pkv], writes=[st_])
                pool.dma(st_d[c], st_[:], reads=[st_], writes=[D_x])
        fw.barrier()

    def phase_B2(l):
        with ExitStack() as es:
            T = layer_tabs(es, l)
            lg, posb = T["lg"], T["posb"]
            rdec = SB(es, "rdec", [64, 16], F32)
            act.op(lambda e: e.activation(out=rdec[:], in_=lg[0:64, :], func=AF.Exp, scale=posb[0:64, 2:3]), reads=[lg, posb], writes=[rdec])
            ST = SB(es, "ST", [64, 4, 512], F32)
            dve.op(lambda e: e.memset(ST[:], 0.0), writes=[ST])
            NB = 3
            IN = [SB(es, "IN%d" % i, [64, 4, 512], F32) for i in range(NB)]
            DC = [SB(es, "DC%d" % i, [64, 16], F32) for i in range(NB)]
            PV = [SB(es, "PV%d" % i, [64, 4, 512], BF16) for i in range(NB)]
            order_f = list(range(NCH))
            order_b = [1, 0] + list(range(NCH - 1, 1, -1))
            for k in range(NCH):
                i = k % NB
                cf, cbk = order_f[k], order_b[k]
                sp.dma(IN[i][:, 0, :], st_d[cf, :, 0, :], writes=[IN[i]])
                sp.dma(IN[i][:, 1, :], st_d[cbk, :, 1, :], writes=[IN[i]])
                sp.dma(IN[i][:, 2, :], st_d[cf, :, 2, :], writes=[IN[i]])
                sp.dma(IN[i][:, 3, :], st_d[cbk, :, 3, :], writes=[IN[i]])
                sp.dma(DC[i][:, 0:8], cdec_d[cf, :, 0:8], writes=[DC[i]])
                sp.dma(DC[i][:, 8:16], cdec_d[cbk, :, 8:16], writes=[DC[i]])
                act.op(lambda e: e.copy(out=PV[i][:], in_=ST[:]), reads=[ST], writes=[PV[i]])
                pool.dma(prev_d[cf, :, 0, :], PV[i][:, 0, :], reads=[PV[i]], writes=[D_x])
                pool.dma(prev_d[cbk, :, 1, :], PV[i][:, 1, :], reads=[PV[i]], writes=[D_x])
                pool.dma(prev_d[cf, :, 2, :], PV[i][:, 2, :], reads=[PV[i]], writes=[D_x])
                pool.dma(prev_d[cbk, :, 3, :], PV[i][:, 3, :], reads=[PV[i]], writes=[D_x])
                dve.op(lambda e: e.tensor_tensor(out=ST[:, 0:2, :].rearrange("p d (h q) -> p (d h) q", h=8), in0=ST[:, 0:2, :].rearrange("p d (h q) -> p (d h) q", h=8),
                                                 in1=bc_last(DC[i][:], 64), op=ALU.mult), reads=[ST, DC[i]], writes=[ST])
                dve.op(lambda e: e.tensor_tensor(out=ST[:, 2:4, :].rearrange("p d (h q) -> p (d h) q", h=8), in0=ST[:, 2:4, :].rearrange("p d (h q) -> p (d h) q", h=8),
                                                 in1=bc_last(rdec[:], 64), op=ALU.mult), reads=[ST, rdec], writes=[ST])
                dve.op(lambda e: e.tensor_tensor(out=ST[:], in0=ST[:], in1=IN[i][:], op=ALU.add), reads=[ST, IN[i]], writes=[ST])
        fw.barrier()


    def phase_C(l, need_ctx=True):
        with ExitStack() as es:
            T = layer_tabs(es, l)
            lg = T["lg"]
            wbr = SB(es, "wbr", [128, 12, D], BF16)
            wo = SB(es, "wo", [128, 8, D], BF16)
            sp.dma(wbr[:], wbrb[l].rearrange("(kc k) c -> k kc c", k=128), writes=[wbr])
            sp.dma(wo[:], woutb[l].rearrange("(kc k) c -> k kc c", k=128), writes=[wo])
            pwf = SB(es, "pwf", [128, 4, 128], F32)
            pw = SB(es, "pw", [128, 4, 128], BF16)
            sp.dma(pwf[:], pool_w[l].rearrange("g c o -> c g o"), writes=[pwf])
            dve.op(lambda e: e.tensor_copy(out=pw[:], in_=pwf[:]), reads=[pwf], writes=[pw])
            pPf = SB(es, "pPf", [128, 20, 128], F32)
            pP = SB(es, "pP", [128, 4, 5, 128], BF16)
            sp.dma(pPf[:], cin["poolP"].rearrange("t g k u -> t (g k) u"), writes=[pPf])
            dve.op(lambda e: e.tensor_copy(out=pP[:].rearrange("t g k u -> t (g k) u"), in_=pPf[:]), reads=[pPf], writes=[pP])
            rdf = SB(es, "rdf", [128, 5, 128], F32)
            sp.dma(rdf[:], cin["rdiff"][:, :, :], writes=[rdf])
            prow = SB(es, "prow", [128, 2, 128], F32)
            sp.dma(prow[:], cin["prow"][:, :, :], writes=[prow])
            mfb = SB(es, "mfb", [128, 2, 512], F32)
            sp.dma(mfb[:], cin["maskfb"][:, :, :], writes=[mfb])
            eall = SB(es, "eall", [16, 2, 8, 128], F32)
            sp.dma(eall[:], cin["eall"][:, :, :, :], writes=[eall])
            ones16 = SB(es, "ones16", [16, 128], F32)
            dve.op(lambda e: e.memset(ones16[:], 1.0), writes=[ones16])
            g1t = SB(es, "g1t", [128, 2, D], F32)
            for s_ in range(2):
                sp.dma(g1t[:, s_, :], modtab[l, s_, 2], writes=[g1t])
            v8 = SB(es, "v8", [128, 8], F32)
            sp.dma(v8[:], ssd_d[l:l + 1, :].partition_broadcast(128), writes=[v8])
            Dtab = SB(es, "Dtab", [128, 8, 64], F32)
            dve.op(lambda e: e.tensor_copy(out=Dtab[:], in_=bc_last(v8[:], 64)), reads=[v8], writes=[Dtab])
            ngt = SB(es, "ngt", [128, 512], F32)
            sp.dma(ngt[:], ssd_ng[l:l + 1, :].partition_broadcast(128), writes=[ngt])
            psct = SB(es, "psct", [128, 512], F32)
            sp.dma(psct[:], pool_sc[l:l + 1, :].partition_broadcast(128), writes=[psct])
            dcomb = SB(es, "dcomb", [128, 8, 128], F32)
            tA = SB(es, "tA", [128, 128], F32)
            tB = SB(es, "tB", [128, 128], F32)
            for h in range(8):
                act.op(lambda e: e.activation(out=tA[:], in_=rdf[:, 0, :], func=AF.Exp, scale=lg[:, h:h + 1]), reads=[rdf, lg], writes=[tA])
                act.op(lambda e: e.activation(out=tB[:], in_=rdf[:, 1, :], func=AF.Exp, scale=lg[:, 8 + h:9 + h]), reads=[rdf, lg], writes=[tB])
                dve.op(lambda e: e.tensor_tensor(out=tA[:], in0=tA[:], in1=rdf[:, 2, :], op=ALU.mult), reads=[tA, rdf], writes=[tA])
                dve.op(lambda e: e.tensor_tensor(out=tB[:], in0=tB[:], in1=rdf[:, 3, :], op=ALU.mult), reads=[tB, rdf], writes=[tB])
                dve.op(lambda e: e.tensor_tensor(out=tA[:], in0=tA[:], in1=tB[:], op=ALU.add), reads=[tA, tB], writes=[tA])
                dve.op(lambda e: e.tensor_tensor(out=dcomb[:, h, :], in0=tA[:], in1=rdf[:, 4, :], op=ALU.add), reads=[tA, rdf], writes=[dcomb])
            qdec = SB(es, "qdec", [128, 2, 4, 128], F32)
            for d in range(2):
                for hp in range(4):
                    for hh in range(2):
                        h = 2 * hp + hh
                        act.op(lambda e: e.activation(out=qdec[hh * 64:(hh + 1) * 64, d, hp, :], in_=prow[hh * 64:(hh + 1) * 64, d, :], func=AF.Exp,
                                                      scale=lg[hh * 64:(hh + 1) * 64, d * 8 + h:d * 8 + h + 1]), reads=[prow, lg], writes=[qdec])
            Bt = SB(es, "Bt", [128, 128], BF16)
            Ctz = SB(es, "Ctz", [128, 2, 128], BF16)
            dve.op(lambda e: e.memset(Ctz[:], 0.0), writes=[Ctz])
            xd = SB(es, "xdC", [128, 2, 512], BF16)
            c2 = SB(es, "c2C", [128, 48], F32)
            prevb = SB(es, "prevb", [128, 4, 512], BF16)
            Qz = SB(es, "Qz", [128, 2, 4, 128], BF16)
            Kz = SB(es, "Kz", [128, 2, 4, 128], BF16)
            dve.op(lambda e: e.memset(Qz[:], 0.0), writes=[Qz])
            dve.op(lambda e: e.memset(Kz[:], 0.0), writes=[Kz])
            Vc = SB(es, "Vc", [128, 512], BF16)
            zsb = SB(es, "zsb", [128, 512], BF16)
            gsb = SB(es, "gsb", [128, 512], BF16)
            gtb = SB(es, "gtb", [128, 3072], BF16)
            upb = [SB(es, "upb%d" % i, [128, 512], BF16) for i in range(3)]
            Xc = SB(es, "Xc", [128, 512], BF16)
            xr = SB(es, "xr", [128, D], F32)
            nacs = SB(es, "nacs", [128, 16], F32)
            rowsT = SB(es, "rowsT", [16, 2, 128], F32)
            RD = SB(es, "RD", [16, 2, 8, 128], F32)
            Lm = SB(es, "Lm", [128, 8, 128], BF16)
            Mm = SB(es, "Mm", [128, 2, 8, 128], BF16)
            XD = SB(es, "XD", [128, 512], BF16)
            E16 = SB(es, "E16", [128, 16], F32)
            y1 = SB(es, "y1", [128, 512], F32)
            y2 = SB(es, "y2", [128, 512], F32)
            st8 = SB(es, "st8", [128, 4, 8], F32)
            s1 = SB(es, "s1", [128, 1], F32)
            junk = SB(es, "junkC", [128, 512], F32)
            brs = SB(es, "brs", [128, 1536], BF16)
            brT = SB(es, "brT", [128, 12, 128], BF16)
            Qs = SB(es, "Qs", [128, 2, 2, 4, 128], BF16)
            IM = SB(es, "IM", [128, 8, 128], BF16)
            mixT = SB(es, "mixT", [128, 4, 128], BF16)
            acc = SB(es, "acc", [128, D], F32)
            tmpm = SB(es, "tmpm", [128, D], F32)
            accb = SB(es, "accb", [128, D], BF16)
            accT = SB(es, "accT", [128, 8, 128], BF16)
            p2 = [PS(es, "p2%d" % i, [128, 1024]) for i in range(2)]
            p1 = [PS(es, "p1%d" % i, [128, 512]) for i in range(4)]

            def segs_of(c):
                return (0, 2) if c < 2 else (2, NCH)

            for c in (range(0 if need_ctx else 2, NCH) if cstage is None else range(3)):
                r0 = c * 128
                lo, hi = segs_of(c)
                si = 0 if c < 2 else 1
                sp.dma(Bt[:], BCt_d[0:128, r0:r0 + 128], writes=[Bt])
                sp.dma(Ctz[0:64, 0, :], BCt_d[128:192, r0:r0 + 128], writes=[Ctz])
                sp.dma(Ctz[64:128, 1, :], BCt_d[192:256, r0:r0 + 128], writes=[Ctz])
                sp.dma(xd[:], Xdt_d[r0:r0 + 128], writes=[xd])
                sp.dma(c2[:], cs2_d[r0:r0 + 128, :], writes=[c2])
                sp.dma(prevb[0:64], prev_d[c], writes=[prevb])
                sp.dma(prevb[64:128], prev_d[c], writes=[prevb])
                for hh_ in range(2):
                    ps__ = slice(hh_ * 64, (hh_ + 1) * 64)
                    sp.dma(Qz[ps__, hh_], Qt_d[:, r0:r0 + 128].rearrange("(hp p) t -> p hp t", p=128)[ps__], writes=[Qz])
                    sp.dma(Kz[ps__, hh_], Kt_d[:, r0:r0 + 128].rearrange("(hp p) t -> p hp t", p=128)[ps__], writes=[Kz])
                sp.dma(Vc[:], vv_d[r0:r0 + 128, :], writes=[Vc])
                sp.dma(zsb[:], zs_d[r0:r0 + 128, :], writes=[zsb])
                sp.dma(gsb[:], gs_d[r0:r0 + 128, :], writes=[gsb])
                sp.dma(gtb[:], gt_d[r0:r0 + 128, :], writes=[gtb])
                sp.dma(Xc[:], X_d[r0:r0 + 128, :], writes=[Xc])
                sp.dma(xr[:], xres[r0:r0 + 128, :], writes=[xr])
                ups = {}
                for dc in (-1, 0, 1):
                    cc = c + dc
                    if lo <= cc < hi:
                        ub = upb[dc + 1]
                        sp.dma(ub[:], up_d[cc * 128:(cc + 1) * 128, :], writes=[ub])
                        ups[dc] = ub
                if cstage == 0:
                    continue
                pe.op(lambda e: e.transpose(p1[0][0:16, 0:128], c2[:, 0:16], ident_f[:]), reads=[c2, ident_f], writes=[p1[0]])
                dve.op(lambda e: e.tensor_copy(out=rowsT[:, 0, :], in_=p1[0][0:16, 0:128]), reads=[p1[0]], writes=[rowsT])
                dve.op(lambda e: e.tensor_scalar(out=rowsT[:, 1, :], in0=rowsT[:, 0, :], scalar1=-1.0, scalar2=None, op0=ALU.mult), reads=[rowsT], writes=[rowsT])
                dve.op(lambda e: e.tensor_tensor(out=RD[:, 0], in0=eall[:, 0], in1=rowsT[:, 0:1, :].to_broadcast([16, 8, 128]), op=ALU.mult), reads=[eall, rowsT], writes=[RD])
                dve.op(lambda e: e.tensor_tensor(out=RD[:, 1], in0=eall[:, 1], in1=rowsT[:, 1:2, :].to_broadcast([16, 8, 128]), op=ALU.mult), reads=[eall, rowsT], writes=[RD])
                if cstage == 1:
                    continue
                for g in range(2):
                    pe.op(lambda e: e.matmul(p1[1][:, g * 128:(g + 1) * 128], Bt[:], Ctz[:, g, :], start=True, stop=True), reads=[Bt, Ctz], writes=[p1[1]])
                for d in range(2):
                    seg = p2[d]
                    for hf in range(2):
                        o_ = seg[:, hf * 512:(hf + 1) * 512]
                        rd_ = RD[:, d, hf * 4:(hf + 1) * 4, :].rearrange("k h l -> k (h l)")
                        ea_ = eall[:, d, hf * 4:(hf + 1) * 4, :].rearrange("k h l -> k (h l)")
                        pe.op(lambda e: e.matmul(o_, ones16[:], rd_, start=True, stop=False), reads=[ones16, RD], writes=[seg])
                        pe.op(lambda e: e.matmul(o_, rowsT[:, 1 - d, :], ea_, start=False, stop=False), reads=[rowsT, eall], writes=[seg])
                        pe.op(lambda e: e.matmul(o_, ident_f[:], mfb[:, d, :], start=False, stop=True), reads=[ident_f, mfb], writes=[seg])
                    act.op(lambda e: e.activation(out=Lm[:].rearrange("p h l -> p (h l)"), in_=seg[:], func=AF.Exp), reads=[seg], writes=[Lm])
                    scv = p1[1][:, 0:256].rearrange("p (g l) -> p g l", g=2).unsqueeze(2).to_broadcast([128, 2, 4, 128])
                    dve.op(lambda e: e.tensor_tensor(out=Mm[:, d].rearrange("p (g r) l -> p g r l", g=2), in0=Lm[:].rearrange("p (g r) l -> p g r l", g=2), in1=scv, op=ALU.mult),
                           reads=[Lm, p1[1]], writes=[Mm])
                if cstage == 2:
                    continue
                pool.op(lambda e: e.tensor_tensor(out=XD[:].rearrange("p (h q) -> p h q", h=8), in0=Xc[:].rearrange("p (h q) -> p h q", h=8), in1=Dtab[:], op=ALU.mult), reads=[Xc, Dtab], writes=[XD])
                yd = p1[2]
                for h in range(8):
                    hs = slice(h * 64, (h + 1) * 64)
                    pe.op(lambda e: e.matmul(yd[:, hs], Mm[:, 0, h, :], xd[:, 0, hs], start=True, stop=False), reads=[Mm, xd], writes=[yd])
                    pe.op(lambda e: e.matmul(yd[:, hs], Mm[:, 1, h, :], xd[:, 1, hs], start=False, stop=False), reads=[Mm, xd], writes=[yd])
                    pe.op(lambda e: e.matmul(yd[:, hs], ident_b[:], XD[:, hs], start=False, stop=True), reads=[ident_b, XD], writes=[yd])
                yo = p2[0]
                for d in range(2):
                    for h in range(8):
                        g = h // 4
                        pe.op(lambda e: e.matmul(yo[:, d * 512 + h * 64:d * 512 + (h + 1) * 64], Ctz[:, g, :], prevb[:, d, h * 64:(h + 1) * 64], start=True, stop=True),
                              reads=[Ctz, prevb], writes=[yo])
                dve.op(lambda e: e.tensor_copy(out=nacs[:, 0:8], in_=c2[:, 0:8]), reads=[c2], writes=[nacs])
                dve.op(lambda e: e.tensor_tensor(out=nacs[:, 8:16], in0=c2[:, 24:32], in1=c2[:, 8:16], op=ALU.subtract), reads=[c2], writes=[nacs])
                act.op(lambda e: e.activation(out=E16[:], in_=nacs[:], func=AF.Exp), reads=[nacs], writes=[E16])
                dve.op(lambda e: e.tensor_tensor(out=y1[:].rearrange("p (h q) -> p h q", h=8), in0=yo[:, 0:512].rearrange("p (h q) -> p h q", h=8), in1=bc_last(E16[:, 0:8], 64), op=ALU.mult), reads=[yo, E16], writes=[y1])
                dve.op(lambda e: e.tensor_tensor(out=y2[:].rearrange("p (h q) -> p h q", h=8), in0=yo[:, 512:1024].rearrange("p (h q) -> p h q", h=8), in1=bc_last(E16[:, 8:16], 64), op=ALU.mult), reads=[yo, E16], writes=[y2])
                pool.op(lambda e: e.tensor_tensor(out=y1[:], in0=y1[:], in1=y2[:], op=ALU.add), reads=[y1, y2], writes=[y1])
                dve.op(lambda e: e.tensor_tensor(out=y1[:], in0=y1[:], in1=yd[:], op=ALU.add), reads=[y1, yd], writes=[y1])
                pool.op(lambda e: e.tensor_tensor(out=y1[:], in0=y1[:], in1=zsb[:], op=ALU.mult), reads=[y1, zsb], writes=[y1])
                act.op(lambda e: e.activation(out=junk[:], in_=y1[:], func=AF.Square, accum_out=s1[:]), reads=[y1], writes=[junk, s1])
                rsqrt(s1, s1, 1.0 / 512, EPS)
                dve.op(lambda e: e.scalar_tensor_tensor(out=brs[:, 0:512], in0=y1[:], scalar=s1[:, 0:1], in1=ngt[:], op0=ALU.mult, op1=ALU.mult), reads=[y1, s1, ngt], writes=[brs])
                if cstage == 3:
                    continue
                pm_ = p1[3]
                for gi in range(4):
                    gs_ = slice(gi * 128, (gi + 1) * 128)
                    first = (c == lo)
                    last = (c == hi - 1)
                    kcur = 2 if first else (3 if last else 1)
                    terms = [(ups[0], kcur)]
                    if -1 in ups:
                        terms.append((ups[-1], 0))
                    if 1 in ups:
                        terms.append((ups[1], 4))
                    for ti, (ub, kind) in enumerate(terms):
                        pe.op(lambda e: e.matmul(pm_[:, gs_], ub[:, gs_], pP[:, gi, kind, :], start=(ti == 0), stop=(ti == len(terms) - 1)), reads=[ub, pP], writes=[pm_])
                act.op(lambda e: e.copy(out=mixT[:].rearrange("p g t -> p (g t)"), in_=pm_[:]), reads=[pm_], writes=[mixT])
                po_ = p1[0]
                for gi in range(4):
                    pe.op(lambda e: e.matmul(po_[:, gi * 128:(gi + 1) * 128], mixT[:, gi, :], pw[:, gi, :], start=True, stop=True), reads=[mixT, pw], writes=[po_])
                dve.op(lambda e: e.tensor_tensor(out=brs[:, 512:1024], in0=po_[:], in1=psct[:], op=ALU.mult), reads=[po_, psct], writes=[brs])
                if cstage == 4:
                    continue
                for d in range(2):
                    pool.op(lambda e: e.tensor_tensor(out=Qs[:, d].rearrange("p a h t -> p a (h t)"), in0=Qz[:].rearrange("p a h t -> p a (h t)"),
                                                      in1=qdec[:, d].rearrange("p h t -> p (h t)").unsqueeze(1).to_broadcast([128, 2, 512]), op=ALU.mult), reads=[Qz, qdec], writes=[Qs])
                if cstage == 41:
                    continue
                kq = p2[1]
                for h in range(8):
                    hp, hh = h // 2, h % 2
                    pe.op(lambda e: e.matmul(kq[:, h * 128:(h + 1) * 128], Kz[:, hh, hp, :], Qz[:, hh, hp, :], start=True, stop=True), reads=[Kz, Qz], writes=[kq])
                dve.op(lambda e: e.tensor_tensor(out=IM[:].rearrange("p h l -> p (h l)"), in0=kq[:], in1=dcomb[:].rearrange("p h l -> p (h l)"), op=ALU.mult), reads=[kq, dcomb], writes=[IM])
                if cstage == 42:
                    continue
                yr = p1[1]
                for h in range(8):
                    hp, hh = h // 2, h % 2
                    hs = slice(h * 64, (h + 1) * 64)
                    ps_ = slice(hh * 64, (hh + 1) * 64)
                    pe.op(lambda e: e.matmul(yr[:, hs], IM[:, h, :], Vc[:, hs], start=True, stop=False), reads=[IM, Vc], writes=[yr])
                    pe.op(lambda e: e.matmul(yr[:, hs], Qs[:, 0, hh, hp, :], prevb[:, 2, hs], start=False, stop=False), reads=[Qs, prevb], writes=[yr])
                    pe.op(lambda e: e.matmul(yr[:, hs], Qs[:, 1, hh, hp, :], prevb[:, 3, hs], start=False, stop=True), reads=[Qs, prevb], writes=[yr])
                if cstage == 43:
                    continue
                for h in range(8):
                    hs = slice(h * 64, (h + 1) * 64)
                    act.op(lambda e: e.activation(out=junk[:, hs], in_=yr[:, hs], func=AF.Identity, accum_out=st8[:, 0, h:h + 1]), reads=[yr], writes=[junk, st8])
                    act.op(lambda e: e.activation(out=junk[:, hs], in_=yr[:, hs], func=AF.Square, accum_out=st8[:, 1, h:h + 1]), reads=[yr], writes=[junk, st8])
                dve.op(lambda e: e.tensor_scalar(out=st8[:, 0, :], in0=st8[:, 0, :], scalar1=1.0 / 64, scalar2=None, op0=ALU.mult), reads=[st8], writes=[st8])
                dve.op(lambda e: e.tensor_tensor(out=st8[:, 2, :], in0=st8[:, 0, :], in1=st8[:, 0, :], op=ALU.mult), reads=[st8], writes=[st8])
                dve.op(lambda e: e.scalar_tensor_tensor(out=st8[:, 3, :], in0=st8[:, 1, :], scalar=1.0 / 64, in1=st8[:, 2, :], op0=ALU.mult, op1=ALU.subtract), reads=[st8], writes=[st8])
                act.op(lambda e: e.activation(out=st8[:, 3, :], in_=st8[:, 3, :], func=AF.Sqrt, bias=64.0 * EPS, scale=1.0), reads=[st8], writes=[st8])
                dve.op(lambda e: e.reciprocal(out=st8[:, 3, :], in_=st8[:, 3, :]), reads=[st8], writes=[st8])
                if cstage == 44:
                    continue
                dve.op(lambda e: e.tensor_tensor(out=y2[:].rearrange("p (h q) -> p h q", h=8), in0=yr[:].rearrange("p (h q) -> p h q", h=8), in1=bc_last(st8[:, 0, :], 64), op=ALU.subtract), reads=[yr, st8], writes=[y2])
                dve.op(lambda e: e.tensor_tensor(out=y2[:].rearrange("p (h q) -> p h q", h=8), in0=y2[:].rearrange("p (h q) -> p h q", h=8), in1=bc_last(st8[:, 3, :], 64), op=ALU.mult), reads=[y2, st8], writes=[y2])
                pool.op(lambda e: e.tensor_tensor(out=brs[:, 1024:1536], in0=y2[:], in1=gsb[:], op=ALU.mult), reads=[y2, gsb], writes=[brs])
                if "brs_d" in dbg:
                    pool.dma(brs_d[r0:r0 + 128, :], brs[:], reads=[brs], writes=[D_x])
                if cstage == 5:
                    continue
                trp = p2[1][:].bitcast(BF16).rearrange("p (j t) -> p j t", t=128)
                for j in range(12):
                    pe.op(lambda e: e.transpose(trp[:, j, :], brs[:, j * 128:(j + 1) * 128], ident_b[:]), reads=[brs, ident_b], writes=[p2[1]])
                act.op(lambda e: e.copy(out=brT[:], in_=trp[:, 0:12, :]), reads=[p2[1]], writes=[brT])
                for i in range(3):
                    bo = p2[0]
                    for hf in range(2):
                        for kc in range(4):
                            pe.op(lambda e: e.matmul(bo[:, hf * 512:(hf + 1) * 512], brT[:, i * 4 + kc, :], wbr[:, i * 4 + kc, hf * 512:(hf + 1) * 512], start=(kc == 0), stop=(kc == 3)),
                                  reads=[brT, wbr], writes=[bo])
                    if i == 0:
                        dve.op(lambda e: e.tensor_tensor(out=acc[:], in0=bo[:], in1=gtb[:, 0:1024], op=ALU.mult), reads=[bo, gtb], writes=[acc])
                    else:
                        dve.op(lambda e: e.tensor_tensor(out=tmpm[:], in0=bo[:], in1=gtb[:, i * 1024:(i + 1) * 1024], op=ALU.mult), reads=[bo, gtb], writes=[tmpm])
                        pool.op(lambda e: e.tensor_tensor(out=acc[:], in0=acc[:], in1=tmpm[:], op=ALU.add), reads=[acc, tmpm], writes=[acc])
                act.op(lambda e: e.copy(out=accb[:], in_=acc[:]), reads=[acc], writes=[accb])
                for j in range(8):
                    pe.op(lambda e: e.transpose(trp[:, j, :], accb[:, j * 128:(j + 1) * 128], ident_b[:]), reads=[accb, ident_b], writes=[p2[1]])
                act.op(lambda e: e.copy(out=accT[:], in_=trp[:, 0:8, :]), reads=[p2[1]], writes=[accT])
                op_ = p2[0]
                for hf in range(2):
                    for kc in range(8):
                        pe.op(lambda e: e.matmul(op_[:, hf * 512:(hf + 1) * 512], accT[:, kc, :], wo[:, kc, hf * 512:(hf + 1) * 512], start=(kc == 0), stop=(kc == 7)), reads=[accT, wo], writes=[op_])
                dve.op(lambda e: e.tensor_tensor(out=tmpm[:], in0=op_[:], in1=g1t[:, si, :], op=ALU.mult), reads=[op_, g1t], writes=[tmpm])
                pool.op(lambda e: e.tensor_tensor(out=xr[:], in0=xr[:], in1=tmpm[:], op=ALU.add), reads=[xr, tmpm], writes=[xr])
                pool.dma(xres[r0:r0 + 128, :], xr[:], reads=[xr], writes=[D_x])
        fw.barrier()


    def phase_D(l, need_ctx=True):
        moe = (l % 2 == 1)
        idx = l // 2
        F_ = EFF if moe else FF
        NJ = F_ // 128
        NEXP = NE if moe else 1
        with ExitStack() as es:
            tabs = SB(es, "tabsD", [128, 3, D], F32)
            xb = SB(es, "xbD", [128, 4, D], F32)
            hn = [SB(es, "hn%d" % i, [128, D], F32) for i in range(2)]
            junk = SB(es, "junkD", [128, D], BF16)
            ssq = [SB(es, "ssq%d" % i, [128, 1], F32) for i in range(2)]
            h2T = SB(es, "h2T", [128, 8, 512], BF16)
            hT32 = SB(es, "hT32", [128, 8, 128], F32)
            aT = SB(es, "aT", [128, NJ, 512], BF16)
            slab = [SB(es, "slab%d" % i, [128, 8, 256], BF16) for i in range(3)]
            W2 = SB(es, "W2", [128, NJ, D], BF16)
            sg = [SB(es, "sg%d" % i, [128, 512], F32) for i in range(2)]
            tmp = [SB(es, "tmpD%d" % i, [128, 512], F32) for i in range(2)]
            pg = [PS(es, "pg%d" % i, [128, 512]) for i in range(2)]
            pu = [PS(es, "pu%d" % i, [128, 512]) for i in range(2)]
            po = [PS(es, "po%d" % i, [128, 512]) for i in range(2)]
            ptr = PS(es, "ptrD", [128, 512])
            plog = PS(es, "plog", [128, 512])
            if moe:
                yacc = SB(es, "yacc", [128, 4, D], F32)
                rw = SB(es, "rw", [128, 4, 8], F32)
                Wr = SB(es, "Wr", [128, 8, 8], F32)
                sp.dma(Wr[:], moe_router[idx].rearrange("(kc k) e -> k kc e", k=128), writes=[Wr])
                lgt = SB(es, "lgt", [128, 8], F32)
                m8 = SB(es, "m8", [128, 8], F32)
                nm = SB(es, "nm", [128, 1], F32)
                mk = SB(es, "mk", [128, 8], F32)
                ex = SB(es, "ex", [128, 8], F32)
                ssum = SB(es, "ssum", [128, 1], F32)
            blocks = ([(0, 256)] if need_ctx else []) + [(256 + 512 * j, 512) for j in range(16)]
            cur_set = None
            nslab = 0
            for (tok0, n) in blocks:
                si = 0 if tok0 < CTX else 1
                if si != cur_set:
                    for q in range(3):
                        sp.dma(tabs[:, q, :], modtab[l, si, 3 + q], writes=[tabs])
                    cur_set = si
                ntt = n // 128
                for tt in range(ntt):
                    r0 = tok0 + tt * 128
                    i = tt % 2
                    sp.dma(xb[:, tt, :], xres[r0:r0 + 128, :], writes=[xb])
                    act.op(lambda e: e.activation(out=junk[:], in_=xb[:, tt, :], func=AF.Square, accum_out=ssq[i][:]), reads=[xb], writes=[junk, ssq[i]])
                    rsqrt(ssq[i], ssq[i], 1.0 / D, EPS)
                    dve.op(lambda e: e.scalar_tensor_tensor(out=hn[i][:], in0=xb[:, tt, :], scalar=ssq[i][:, 0:1], in1=tabs[:, 1, :], op0=ALU.mult, op1=ALU.mult), reads=[xb, ssq[i], tabs], writes=[hn[i]])
                    pool.op(lambda e: e.tensor_tensor(out=hn[i][:], in0=hn[i][:], in1=tabs[:, 0, :], op=ALU.add), reads=[hn[i], tabs], writes=[hn[i]])
                    for hf in range(2):
                        for k4 in range(4):
                            kc = hf * 4 + k4
                            pe.op(lambda e: e.transpose(ptr[:, k4 * 128:(k4 + 1) * 128], hn[i][:, kc * 128:(kc + 1) * 128], ident_f[:]), reads=[hn[i], ident_f], writes=[ptr])
                        dve.op(lambda e: e.tensor_copy(out=h2T[:, hf * 4:(hf + 1) * 4, tt * 128:(tt + 1) * 128], in_=ptr[:].rearrange("p (a t) -> p a t", a=4)), reads=[ptr], writes=[h2T])
                        if moe:
                            act.op(lambda e: e.copy(out=hT32[:, hf * 4:(hf + 1) * 4, :], in_=ptr[:].rearrange("p (a t) -> p a t", a=4)), reads=[ptr], writes=[hT32])
                    if moe:
                        for kc in range(8):
                            pe.op(lambda e: e.matmul(plog[:, 0:8], hT32[:, kc, :], Wr[:, kc, :], start=(kc == 0), stop=(kc == 7)), reads=[hT32, Wr], writes=[plog])
                        dve.op(lambda e: e.tensor_copy(out=lgt[:], in_=plog[:, 0:8]), reads=[plog], writes=[lgt])
                        dve.op(lambda e: e.max(out=m8[:], in_=lgt[:]), reads=[lgt], writes=[m8])
                        dve.op(lambda e: e.tensor_scalar(out=nm[:], in0=m8[:, 0:1], scalar1=-1.0, scalar2=None, op0=ALU.mult), reads=[m8], writes=[nm])
                        dve.op(lambda e: e.tensor_scalar(out=mk[:], in0=lgt[:], scalar1=m8[:, 1:2], scalar2=None, op0=ALU.is_ge), reads=[lgt, m8], writes=[mk])
                        act.op(lambda e: e.activation(out=ex[:], in_=lgt[:], func=AF.Exp, bias=nm[:, 0:1], scale=1.0), reads=[lgt, nm], writes=[ex])
                        dve.op(lambda e: e.tensor_tensor(out=ex[:], in0=ex[:], in1=mk[:], op=ALU.mult), reads=[ex, mk], writes=[ex])
                        act.op(lambda e: e.activation(out=mk[:], in_=ex[:], func=AF.Identity, accum_out=ssum[:]), reads=[ex], writes=[mk, ssum])
                        dve.op(lambda e: e.reciprocal(out=ssum[:], in_=ssum[:]), reads=[ssum], writes=[ssum])
                        dve.op(lambda e: e.tensor_scalar(out=rw[:, tt, :], in0=ex[:], scalar1=ssum[:, 0:1], scalar2=None, op0=ALU.mult), reads=[ex, ssum], writes=[rw])
                for e_ in range(NEXP if not (moe and dstage in ('r', 'e1', 'e2', 'e4')) else {'r': 0, 'e1': 1, 'e2': 2, 'e4': 4}[dstage]):
                    slabs = m13s[idx, e_] if moe else f13s[idx]
                    w2src = m2b[idx, e_] if moe else f2b[idx]
                    for j in range(NJ):
                        sl = slab[nslab % 3]
                        nslab += 1
                        sp.dma(sl[:], slabs[j], writes=[sl])
                        if j == 2:
                            for ja in range(0, NJ, 7):
                                jb = min(ja + 7, NJ)
                                sp.dma(W2[:, ja:jb, :], w2src[ja * 128:jb * 128, :].rearrange("(j k) c -> k j c", k=128), writes=[W2])
                        g_, u_ = pg[j % 2], pu[j % 2]
                        for kc in range(8):
                            pe.op(lambda e: e.matmul(g_[:, 0:n], sl[:, kc, 0:128], h2T[:, kc, 0:n], start=(kc == 0), stop=(kc == 7)), reads=[sl, h2T], writes=[g_])
                        for kc in range(8):
                            pe.op(lambda e: e.matmul(u_[:, 0:n], sl[:, kc, 128:256], h2T[:, kc, 0:n], start=(kc == 0), stop=(kc == 7)), reads=[sl, h2T], writes=[u_])
                        s_ = sg[j % 2]
                        act.op(lambda e: e.activation(out=s_[:, 0:n], in_=g_[:, 0:n], func=AF.Silu), reads=[g_], writes=[s_])
                        dve.op(lambda e: e.tensor_tensor(out=aT[:, j, 0:n], in0=s_[:, 0:n], in1=u_[:, 0:n], op=ALU.mult), reads=[s_, u_], writes=[aT])
                    for tt in range(ntt):
                        for hf in range(2):
                            p_ = po[(tt * 2 + hf) % 2]
                            cs_ = slice(hf * 512, (hf + 1) * 512)
                            for j in range(NJ):
                                pe.op(lambda e: e.matmul(p_[:], aT[:, j, tt * 128:(tt + 1) * 128], W2[:, j, cs_], start=(j == 0), stop=(j == NJ - 1)), reads=[aT, W2], writes=[p_])
                            if moe:
                                if e_ == 0:
                                    dve.op(lambda e: e.tensor_scalar(out=yacc[:, tt, cs_], in0=p_[:], scalar1=rw[:, tt, 0:1], scalar2=None, op0=ALU.mult), reads=[p_, rw], writes=[yacc])
                                else:
                                    dve.op(lambda e: e.scalar_tensor_tensor(out=yacc[:, tt, cs_], in0=p_[:], scalar=rw[:, tt, e_:e_ + 1], in1=yacc[:, tt, cs_], op0=ALU.mult, op1=ALU.add),
                                           reads=[p_, rw, yacc], writes=[yacc])
                            else:
                                t_ = tmp[hf]
                                dve.op(lambda e: e.tensor_tensor(out=t_[:], in0=p_[:], in1=tabs[:, 2, cs_], op=ALU.mult), reads=[p_, tabs], writes=[t_])
                                pool.op(lambda e: e.tensor_tensor(out=xb[:, tt, cs_], in0=xb[:, tt, cs_], in1=t_[:], op=ALU.add), reads=[xb, t_], writes=[xb])
                    if moe:
                        fw.barrier()
                for tt in range(ntt):
                    r0 = tok0 + tt * 128
                    if moe and dstage != 'r':
                        dve.op(lambda e: e.tensor_tensor(out=yacc[:, tt, :], in0=yacc[:, tt, :], in1=tabs[:, 2, :], op=ALU.mult), reads=[yacc, tabs], writes=[yacc])
                        pool.op(lambda e: e.tensor_tensor(out=xb[:, tt, :], in0=xb[:, tt, :], in1=yacc[:, tt, :], op=ALU.add), reads=[xb, yacc], writes=[xb])
                    pool.dma(xres[r0:r0 + 128, :], xb[:, tt, :], reads=[xb], writes=[D_x])
        fw.barrier()

    def phase_final():
        with ExitStack() as es:
            gt = SB(es, "gtF", [128, D], F32)
            sp.dma(gt[:], fin_g[0:1, :].partition_broadcast(128), writes=[gt])
            xt = [SB(es, "xtF%d" % i, [128, D], F32) for i in range(3)]
            ot = [SB(es, "otF%d" % i, [128, D], F32) for i in range(2)]
            junk = SB(es, "junkF", [128, D], BF16)
            ssq = [SB(es, "ssqF%d" % i, [128, 1], F32) for i in range(2)]
            for c in range(2, NCH):
                x_ = xt[c % 3]
                i = c % 2
                sp.dma(x_[:], xres[c * 128:(c + 1) * 128, :], writes=[x_])
                act.op(lambda e: e.activation(out=junk[:], in_=x_[:], func=AF.Square, accum_out=ssq[i][:]), reads=[x_], writes=[junk, ssq[i]])
                rsqrt(ssq[i], ssq[i], 1.0 / D, EPS)
                dve.op(lambda e: e.scalar_tensor_tensor(out=ot[i][:], in0=x_[:], scalar=ssq[i][:, 0:1], in1=gt[:], op0=ALU.mult, op1=ALU.mult), reads=[x_, ssq[i], gt], writes=[ot[i]])
                pool.dma(yout[(c - 2) * 128:(c - 1) * 128, :], ot[i][:], reads=[ot[i]], writes=[D_x])
        fw.barrier()

    K.nc = nc
    K.fw = fw
    layers = list(range(depth))
    phase_cast(layers)
    phase_mod(layers)
    for l in layers:
        last = (l == DEPTH - 1)
        phase_A0(l)
        if upto == 'A0' and l == upto_layer:
            break
        phase_A1(l)
        if upto == 'A1' and l == upto_layer:
            break
        phase_B1(l)
        phase_B2(l)
        if upto == 'B2' and l == upto_layer:
            break
        phase_C(l, need_ctx=not last)
        if upto == 'C' and l == upto_layer:
            break
        phase_D(l, need_ctx=not last)
    if upto is None:
        phase_final()
    fw.barrier()
    es_all.close()
    return nc


def make_in_map(I, b, depth=DEPTH):
    cs = host_consts()
    m = {}
    m["xin"] = np.ascontiguousarray(np.concatenate([I["ctx"][b], I["x"][b]], axis=0))
    m["cvec"] = np.ascontiguousarray(np.stack([I["c_ctx"], I["c"][b]], axis=0))
    for k in ("ada_w", "ada_b", "norm1_g", "norm2_g", "w_in", "ssd_conv_w", "ssd_conv_b", "ssd_d", "ssd_norm_g", "pool_w",
              "pool_scale", "w_out", "ffn_w13", "ffn_w2", "moe_router", "moe_w13", "moe_w2"):
        m[k] = np.ascontiguousarray(I[k])
    m["ssd_dt_bias"] = np.ascontiguousarray(I["ssd_dt_bias"].reshape(DEPTH, 16))
    m["ssd_a_log"] = np.ascontiguousarray(I["ssd_a_log"].reshape(DEPTH, 16))
    m["ret_decay_logit"] = np.ascontiguousarray(I["ret_decay_logit"].reshape(DEPTH, 16))
    m["w_branch"] = np.ascontiguousarray(I["w_branch"].reshape(DEPTH, 1536, D))
    m["final_norm_g"] = np.ascontiguousarray(I["final_norm_g"].reshape(1, D))
    if depth // 2 == 0:
        m["moe_w13"] = np.zeros((1, 1, 128, 128), np.float32)
        m["moe_w2"] = np.zeros((1, 1, 128, 128), np.float32)
    for k, v in cs.items():
        m["c_" + k] = np.ascontiguousarray(v)
    return m


def kernel(**inputs):
    nc = build()
    in_maps = [make_in_map(inputs, b) for b in range(4)]
    res = run_bass_kernel_spmd(nc, in_maps, core_ids=[0, 1, 2, 3])
    return np.stack([np.asarray(res.results[b]["yout"]) for b in range(4)], axis=0).astype(np.float32)
```

```python
import math
from contextlib import ExitStack
import numpy as np
import ml_dtypes
import concourse.bass as bass
import concourse.mybir as mybir
from concourse.bass_utils import run_bass_kernel_spmd

F32 = mybir.dt.float32
BF16 = mybir.dt.bfloat16
AF = mybir.ActivationFunctionType
ALU = mybir.AluOpType
AX = mybir.AxisListType

D = 1024
SEQ = 8192
CTX = 256
S = SEQ + CTX
NCH = S // 128
DEPTH = 4
EPS = 1e-6
FF = 2816
EFF = 3584
NE = 8
NEXT = 7952
C_Z, C_XBC, C_DT, C_POOL, C_Q, C_K, C_V, C_G, C_GATE, C_QP, C_KP = 0, 512, 1280, 1296, 1808, 2320, 2832, 3344, 3856, 6928, 7440
HPAD = S + 8
NEG = -30000.0


def seg_of(tok0):
    return tok0 + 2 if tok0 < CTX else tok0 + 6


class Buf:
    __slots__ = ("name", "t", "w", "rs", "dsem", "ps")

    def __init__(self, name, t=None):
        self.name = name
        self.t = t
        self.w = None
        self.rs = []
        self.dsem = None
        self.ps = False

    def __getitem__(self, k):
        return self.t[k]


class Sem:
    __slots__ = ("h", "n", "mult")

    def __init__(self, h, mult):
        self.h = h
        self.n = 0
        self.mult = mult


class Eng:
    def __init__(self, fw, e, name):
        self.fw = fw
        self.e = e
        self.name = name
        self.sem = Sem(fw.nc.alloc_semaphore("s_" + name), 1)
        self.seen = {}
        self.selfsync = name in ("act", "dve", "pool")

    def _wait(self, dep, own=False):
        if dep is None:
            return
        s, cnt, g = dep
        if g != self.fw.gen:
            return
        if s is self.sem and not (own and self.selfsync):
            return
        if self.seen.get(id(s), 0) >= cnt:
            return
        self.e.wait_ge(s.h, cnt * s.mult)
        self.seen[id(s)] = cnt

    def deps(self, reads, writes):
        for b in reads:
            self._wait(b.w, own=True)
            if b.ps:
                for r in b.rs:
                    self._wait(r)
        for b in writes:
            self._wait(b.w, own=True)
            for r in b.rs:
                self._wait(r)

    def mark(self, reads, writes, tok):
        for b in reads:
            b.rs.append(tok)
            if len(b.rs) > 8:
                d = {}
                for s, c, g in b.rs:
                    if g != self.fw.gen:
                        continue
                    if id(s) not in d or d[id(s)][1] < c:
                        d[id(s)] = (s, c, g)
                b.rs = list(d.values())
        for b in writes:
            b.w = tok
            b.rs = []

    def op(self, fn, reads=(), writes=()):
        self.deps(reads, writes)
        ins = fn(self.e)
        self.sem.n += 1
        ins.then_inc(self.sem.h, 1)
        self.mark(reads, writes, (self.sem, self.sem.n, self.fw.gen))
        return ins

    def dma(self, out, in_, reads=(), writes=(), **kw):
        self.deps(reads, writes)
        key = None
        for b in list(writes) + list(reads):
            if b.t is not None:
                key = b
                break
        if key is None:
            key = writes[0]
        if key.dsem is None:
            if self.fw.free_dsems:
                key.dsem = self.fw.free_dsems.pop()
            else:
                key.dsem = Sem(self.fw.nc.alloc_semaphore("d_" + key.name), 16)
                self.fw.dsems.append(key.dsem)
            self.fw.live_dsems.append(key.dsem)
        ds = key.dsem
        ins = self.e.dma_start(out=out, in_=in_, **kw)
        ds.n += 1
        ins.then_inc(ds.h, 16)
        self.mark(reads, writes, (ds, ds.n, self.fw.gen))
        return ins


class FW:
    def __init__(self, nc):
        self.nc = nc
        self.dsems = []
        self.free_dsems = []
        self.live_dsems = []
        self.pe = Eng(self, nc.tensor, "pe")
        self.act = Eng(self, nc.scalar, "act")
        self.dve = Eng(self, nc.vector, "dve")
        self.pool = Eng(self, nc.gpsimd, "pool")
        self.sp = Eng(self, nc.sync, "sp")
        self.engs = [self.pe, self.act, self.dve, self.pool, self.sp]
        self.gen = 0
        self.epoch = 0
        self.semE = nc.alloc_semaphore("s_epochE")
        self.semG = nc.alloc_semaphore("s_epochG")

    def barrier(self):
        for e in self.engs:
            for o in self.engs:
                if o is e:
                    if o.sem.n > 0:
                        e.e.wait_ge(o.sem.h, o.sem.n)
                else:
                    e._wait((o.sem, o.sem.n, self.gen))
            for ds in self.dsems:
                e._wait((ds, ds.n, self.gen))
        self.free_dsems.extend(self.live_dsems)
        self.live_dsems = []


def host_consts():
    c = {}
    c["ident"] = np.eye(128, dtype=np.float32)
    k = np.arange(128)
    c["triu"] = (k[:, None] <= k[None, :]).astype(np.float32)
    c["ones"] = np.ones((128, 128), np.float32)
    mf = np.where(k[None, :] >= k[:, None], 0.0, NEG).astype(np.float32)
    mb = np.where(k[:, None] >= k[None, :], 0.0, NEG).astype(np.float32)
    c["maskfb"] = np.stack([np.tile(mf, (1, 4)), np.tile(mb, (1, 4))], axis=1).astype(np.float32)
    e = np.zeros((16, 2, 8, 128), np.float32)
    for d in range(2):
        for h in range(8):
            e[d * 8 + h, d, h, :] = 1.0
    c["eall"] = e
    dp = np.maximum(k[None, :] - k[:, None], 0).astype(np.float32)
    dn = np.maximum(k[:, None] - k[None, :], 0).astype(np.float32)
    mu = (k[None, :] > k[:, None]).astype(np.float32)
    ml = (k[:, None] > k[None, :]).astype(np.float32)
    c["rdiff"] = np.stack([dp, dn, mu, ml, 2.0 * np.eye(128, dtype=np.float32)], axis=1)
    pos = np.stack([127.0 - k, k.astype(np.float64), np.full(128, 128.0)], axis=1).astype(np.float32)
    c["pos"] = pos
    prow = np.stack([np.tile((k + 1.0)[None, :], (128, 1)), np.tile((128.0 - k)[None, :], (128, 1))], axis=1).astype(np.float32)
    c["prow"] = prow
    P = np.zeros((128, 4, 5, 128), np.float32)
    for gi, w in enumerate((2, 4, 8, 16)):
        left = w // 2
        right = w - 1 - left
        for t in range(128):
            for tp in range(t - left, t + right + 1):
                v = 1.0 / w
                if tp < 0:
                    P[tp + 128, gi, 0, t] += v
                elif tp >= 128:
                    P[tp - 128, gi, 4, t] += v
                else:
                    P[tp, gi, 1, t] += v
            P[t, gi, 1, t] -= 1.0
            lo = max(t - left, 0)
            hi = t + right
            cnt = hi - lo + 1
            for tp in range(lo, min(hi, 127) + 1):
                P[tp, gi, 2, t] += 1.0 / cnt
            P[t, gi, 2, t] -= 1.0
            lo = t - left
            hi = min(t + right, 127)
            cnt = hi - lo + 1
            for tp in range(max(lo, 0), hi + 1):
                P[tp, gi, 3, t] += 1.0 / cnt
            P[t, gi, 3, t] -= 1.0
    c["poolP"] = P
    inv = 10000.0 ** (-np.arange(16, dtype=np.float64) / 16)
    t = np.arange(SEQ)
    row = (t // 64).astype(np.float64)
    col = (t % 64).astype(np.float64)
    ang = np.concatenate([row[:, None] * inv[None, :], col[:, None] * inv[None, :]], axis=1)
    cos = np.cos(ang)
    sin = np.sin(ang)
    cos64 = np.concatenate([cos, cos], axis=1)
    sin64 = np.concatenate([-sin, sin], axis=1)
    cosT = np.ones((128, S), np.float32)
    sinT = np.zeros((128, S), np.float32)
    cosT[:, CTX:] = np.tile(cos64.T, (2, 1))
    sinT[:, CTX:] = np.tile(sin64.T, (2, 1))
    c["ropec"] = cosT
    c["ropes"] = sinT
    return c


CONST_SHAPES = None


class K:
    pass


def build(depth=DEPTH, upto=None, dbg=(), cstage=None, upto_layer=0, dstage=None):
    nc = bass.Bass("TRN2", target_bir_lowering=False)
    fw = FW(nc)
    pe, act, dve, pool, sp = fw.pe, fw.act, fw.dve, fw.pool, fw.sp
    cs = host_consts()

    def din(name, shape, dt=F32):
        return nc.dram_tensor(name, list(shape), dt, kind="ExternalInput").ap()

    def dscr(name, shape, dt):
        kind = "ExternalOutput" if name in dbg else "Internal"
        return nc.dram_tensor(name, list(shape), dt, kind=kind).ap()

    xin = din("xin", [S, D])
    cvec = din("cvec", [2, D])
    ada_w = din("ada_w", [DEPTH, D, 6 * D])
    ada_b = din("ada_b", [DEPTH, 6 * D])
    norm1_g = din("norm1_g", [DEPTH, D])
    norm2_g = din("norm2_g", [DEPTH, D])
    w_in = din("w_in", [DEPTH, D, 6928])
    conv_w = din("ssd_conv_w", [DEPTH, 5, 768])
    conv_b = din("ssd_conv_b", [DEPTH, 768])
    dt_bias = din("ssd_dt_bias", [DEPTH, 16])
    a_log = din("ssd_a_log", [DEPTH, 16])
    ssd_d = din("ssd_d", [DEPTH, 8])
    ssd_ng = din("ssd_norm_g", [DEPTH, 512])
    pool_w = din("pool_w", [DEPTH, 4, 128, 128])
    pool_sc = din("pool_scale", [DEPTH, 512])
    ret_dl = din("ret_decay_logit", [DEPTH, 16])
    w_branch = din("w_branch", [DEPTH, 1536, D])
    w_out = din("w_out", [DEPTH, D, D])
    ffn_w13 = din("ffn_w13", [2, D, 2 * FF])
    ffn_w2 = din("ffn_w2", [2, FF, D])
    moe_router = din("moe_router", [2, D, NE])
    nmoe = max(depth // 2, 0)
    moe_w13 = din("moe_w13", [2, NE, D, 2 * EFF] if nmoe else [1, 1, 128, 128])
    moe_w2 = din("moe_w2", [2, NE, EFF, D] if nmoe else [1, 1, 128, 128])
    fin_g = din("final_norm_g", [1, D])
    cin = {k: din("c_" + k, v.shape) for k, v in cs.items()}
    yout = nc.dram_tensor("yout", [SEQ, D], F32, kind="ExternalOutput").ap()

    xres = dscr("xres", [S, D], F32)
    winb = dscr("winb", [DEPTH, D, NEXT], BF16)
    wbrb = dscr("wbrb", [DEPTH, 1536, D], BF16)
    woutb = dscr("woutb", [DEPTH, D, D], BF16)
    f13s = dscr("f13s", [2, FF // 128, 128, 8, 256], BF16)
    f2b = dscr("f2b", [2, FF, D], BF16)
    m13s = dscr("m13s", [2, NE, EFF // 128, 128, 8, 256] if depth // 2 else [1, 1, 1, 128, 8, 256], BF16)
    m2b = dscr("m2b", [2, NE, EFF, D] if depth // 2 else [1, 1, 128, 128], BF16)
    modtab = dscr("modtab", [DEPTH, 2, 6, 128, D], F32)
    hTd = dscr("hTd", [D, HPAD], BF16)
    zs_d = dscr("zs_d", [S, 512], BF16)
    gt_d = dscr("gt_d", [S, 3072], BF16)
    up_d = dscr("up_d", [S, 512], BF16)
    vv_d = dscr("vv_d", [S, 512], BF16)
    gs_d = dscr("gs_d", [S, 512], BF16)
    X_d = dscr("X_d", [S, 512], BF16)
    Btm_d = dscr("Btm_d", [S, 128], BF16)
    BCt_d = dscr("BCt_d", [256, S], BF16)
    dtr_d = dscr("dtr_d", [S, 16], F32)
    Qt_d = dscr("Qt_d", [512, S], BF16)
    Kt_d = dscr("Kt_d", [512, S], BF16)
    Ktm_d = dscr("Ktm_d", [S, 512], BF16)
    Xdt_d = dscr("Xdt_d", [S, 2, 512], BF16)
    cs2_d = dscr("cs2_d", [S, 48], F32)
    cdec_d = dscr("cdec_d", [NCH, 64, 16], F32)
    st_d = dscr("st_d", [NCH, 64, 4, 512], F32)
    prev_d = dscr("prev_d", [NCH, 64, 4, 512], BF16)
    D_x = Buf("dram")

    es_all = ExitStack()

    uniq = [0]

    def SB(es, name, shape, dt):
        uniq[0] += 1
        name = "%s_%d" % (name, uniq[0])
        return Buf(name, es.enter_context(nc.sbuf_tensor(name, list(shape), dt)))

    def PS(es, name, shape, dt=F32):
        uniq[0] += 1
        name = "%s_%d" % (name, uniq[0])
        b = Buf(name, es.enter_context(nc.psum_tensor(name, list(shape), dt)))
        b.ps = True
        return b

    ident_f = SB(es_all, "ident_f", [128, 128], F32)
    ident_b = SB(es_all, "ident_b", [128, 128], BF16)
    sp.dma(ident_f[:], cin["ident"][:, :], writes=[ident_f])
    dve.op(lambda e: e.tensor_copy(out=ident_b[:], in_=ident_f[:]), reads=[ident_f], writes=[ident_b])

    rr = [0]

    def cast_eng():
        rr[0] += 1
        return (dve, pool, act)[rr[0] % 3]

    def do_copy(eng, out, in_, reads, writes):
        if eng is act:
            return eng.op(lambda e: e.copy(out=out, in_=in_), reads=reads, writes=writes)
        return eng.op(lambda e: e.tensor_copy(out=out, in_=in_), reads=reads, writes=writes)

    def phase_cast(layers):
        with ExitStack() as es:
            NB = 3
            CW = 2048
            fbuf = [SB(es, "cf%d" % i, [128, CW], F32) for i in range(NB)]
            bbuf = [SB(es, "cb%d" % i, [128, CW], BF16) for i in range(NB)]
            it = [0]

            def cast2d(src, dst, rows, cols, perm_qk=False):
                for r0 in range(0, rows, 128):
                    for c0 in range(0, cols, CW):
                        cw = min(CW, cols - c0)
                        i = it[0] % NB
                        it[0] += 1
                        sp.dma(fbuf[i][:, 0:cw], src[r0:r0 + 128, c0:c0 + cw], writes=[fbuf[i]])
                        do_copy(cast_eng(), bbuf[i][:, 0:cw], fbuf[i][:, 0:cw], [fbuf[i]], [bbuf[i]])
                        pool.dma(dst[r0:r0 + 128, c0:c0 + cw], bbuf[i][:, 0:cw], reads=[bbuf[i]], writes=[D_x])

            def cast13(src, dst, F_):
                cwh = F_ // 2
                nj = cwh // 128
                for kc in range(8):
                    for q in range(4):
                        i = it[0] % NB
                        it[0] += 1
                        two, j0 = q // 2, (q % 2) * nj
                        sp.dma(fbuf[i][:, 0:cwh], src[kc * 128:(kc + 1) * 128, q * cwh:(q + 1) * cwh], writes=[fbuf[i]])
                        do_copy(cast_eng(), bbuf[i][:, 0:cwh], fbuf[i][:, 0:cwh], [fbuf[i]], [bbuf[i]])
                        for ja in range(0, nj, 7):
                            jb = min(ja + 7, nj)
                            pool.dma(dst[j0 + ja:j0 + jb, :, kc, two * 128:(two + 1) * 128].rearrange("j k i -> k j i"),
                                     bbuf[i][:, ja * 128:jb * 128].rearrange("k (j i) -> k j i", i=128), reads=[bbuf[i]], writes=[D_x])

            for l in layers:
                cast2d(w_in[l], winb[l], D, 6928)
                for r0 in range(0, D, 128):
                    i = it[0] % NB
                    it[0] += 1
                    sp.dma(fbuf[i][:, 0:1024], w_in[l][r0:r0 + 128, C_Q:C_Q + 1024], writes=[fbuf[i]])
                    srcv = fbuf[i][:, 0:1024].rearrange("p (h two j) -> p h two j", two=2, j=32)
                    dstv = bbuf[i][:, 0:1024].rearrange("p (h two j) -> p h two j", two=2, j=32)
                    dve.op(lambda e: e.tensor_copy(out=dstv[:, :, 0, :], in_=srcv[:, :, 1, :]), reads=[fbuf[i]], writes=[bbuf[i]])
                    dve.op(lambda e: e.tensor_copy(out=dstv[:, :, 1, :], in_=srcv[:, :, 0, :]), reads=[fbuf[i]], writes=[bbuf[i]])
                    pool.dma(winb[l][r0:r0 + 128, C_QP:C_QP + 1024], bbuf[i][:, 0:1024], reads=[bbuf[i]], writes=[D_x])
                cast2d(w_branch[l], wbrb[l], 1536, D)
                cast2d(w_out[l], woutb[l], D, D)
                if l % 2 == 0:
                    cast13(ffn_w13[l // 2], f13s[l // 2], FF)
                    cast2d(ffn_w2[l // 2], f2b[l // 2], FF, D)
                elif dstage != 'nomoecast':
                    for e_ in range(NE):
                        cast13(moe_w13[l // 2][e_], m13s[l // 2][e_], EFF)
                        cast2d(moe_w2[l // 2][e_], m2b[l // 2][e_], EFF, D)
        fw.barrier()

    def phase_mod(layers):
        with ExitStack() as es:
            cv = SB(es, "cv", [128, 2, 8], F32)
            scb = SB(es, "scb", [128, 2, 8, 128], F32)
            wbuf = [SB(es, "aw%d" % i, [128, 8, 512], F32) for i in range(2)]
            bb = [SB(es, "ab%d" % i, [128, 512], F32) for i in range(2)]
            ng = SB(es, "ng", [128, 2, D], F32)
            ot = [SB(es, "mo%d" % i, [128, 512], F32) for i in range(2)]
            pm = [PS(es, "pm%d" % i, [128, 512]) for i in range(2)]
            with nc.allow_non_contiguous_dma(reason="tiny"):
                for s_ in range(2):
                    sp.dma(cv[:, s_, :], cvec[s_].rearrange("(kc k) -> k kc", k=128), writes=[cv])
            act.op(lambda e: e.activation(out=cv[:], in_=cv[:], func=AF.Silu), reads=[cv], writes=[cv])
            for s_ in range(2):
                dve.op(lambda e: e.tensor_copy(out=scb[:, s_], in_=cv[:, s_, :].unsqueeze(2).to_broadcast([128, 8, 128])),
                       reads=[cv], writes=[scb])
            n = 0
            for l in layers:
                sp.dma(ng[:, 0, :], norm1_g[l:l + 1, :].partition_broadcast(128), writes=[ng])
                sp.dma(ng[:, 1, :], norm2_g[l:l + 1, :].partition_broadcast(128), writes=[ng])
                for cb in range(12):
                    wi = n % 2
                    n += 1
                    sp.dma(wbuf[wi][:], ada_w[l][:, cb * 512:(cb + 1) * 512].rearrange("(kc k) c -> k kc c", k=128), writes=[wbuf[wi]])
                    sp.dma(bb[wi][:], ada_b[l:l + 1, cb * 512:(cb + 1) * 512].partition_broadcast(128), writes=[bb[wi]])
                    for s_ in range(2):
                        p_ = pm[s_]
                        for kc in range(8):
                            pe.op(lambda e: e.matmul(p_[:], scb[:, s_, kc, :], wbuf[wi][:, kc, :], start=(kc == 0), stop=(kc == 7)),
                                  reads=[scb, wbuf[wi]], writes=[p_])
                        o_ = ot[s_]
                        which = cb // 2
                        half = cb % 2
                        dve.op(lambda e: e.tensor_tensor(out=o_[:], in0=p_[:], in1=bb[wi][:], op=ALU.add), reads=[p_, bb[wi]], writes=[o_])
                        if which in (1, 4):
                            gsel = 0 if which == 1 else 1
                            dve.op(lambda e: e.scalar_tensor_tensor(out=o_[:], in0=o_[:], scalar=1.0, in1=ng[:, gsel, half * 512:(half + 1) * 512],
                                                                    op0=ALU.add, op1=ALU.mult), reads=[o_, ng], writes=[o_])
                        pool.dma(modtab[l, s_, which, :, half * 512:(half + 1) * 512], o_[:], reads=[o_], writes=[D_x])
        fw.barrier()

    def rsqrt(out, in_, scale, bias):
        act.op(lambda e: e.activation(out=out[:], in_=in_[:], func=AF.Sqrt, bias=bias, scale=scale), reads=[in_], writes=[out])
        dve.op(lambda e: e.reciprocal(out=out[:], in_=out[:]), reads=[out], writes=[out])

    def phase_A0(l):
        with ExitStack() as es:
            tabs = SB(es, "tabs", [128, 2, 2, D], F32)
            for s_ in range(2):
                sp.dma(tabs[:, s_, 0, :], modtab[l, s_, 0], writes=[tabs])
                sp.dma(tabs[:, s_, 1, :], modtab[l, s_, 1], writes=[tabs])
            zt = SB(es, "zt", [128, 8, 4], BF16)
            dve.op(lambda e: e.memset(zt[:], 0.0), writes=[zt])
            for kc in range(8):
                for off in (0, 258, 8454):
                    w_ = 2 if off != 258 else 4
                    pool.dma(hTd[kc * 128:(kc + 1) * 128, off:off + w_], zt[:, kc, 0:w_], reads=[zt], writes=[D_x])
            NB = 2
            xt = [SB(es, "xt%d" % i, [128, D], F32) for i in range(3)]
            hb = [SB(es, "hb%d" % i, [128, D], BF16) for i in range(NB)]
            tmp = [SB(es, "tmp%d" % i, [128, D], F32) for i in range(NB)]
            junk = SB(es, "junk", [128, D], F32)
            ss = [SB(es, "ss%d" % i, [128, 1], F32) for i in range(NB)]
            rstd = [SB(es, "rstd%d" % i, [128, 1], F32) for i in range(NB)]
            hT = [SB(es, "hT%d" % i, [128, 8, 512], BF16) for i in range(2)]
            ptr = [PS(es, "ptr%d" % i, [128, 8, 128], BF16) for i in range(2)]
            blocks = [(0, 2)] + [(2 + 4 * j, 4) for j in range(16)]
            first = True
            for bi, (c0, ncb) in enumerate(blocks):
                hTb = hT[bi % 2]
                for ci in range(ncb):
                    c_ = c0 + ci
                    i = c_ % NB
                    x_ = xt[c_ % 3]
                    src = xin if l == 0 else xres
                    sp.dma(x_[:], src[c_ * 128:(c_ + 1) * 128, :], writes=[x_])
                    if l == 0:
                        pool.dma(xres[c_ * 128:(c_ + 1) * 128, :], x_[:], reads=[x_], writes=[D_x])
                    si = 0 if c_ < 2 else 1
                    act.op(lambda e: e.activation(out=junk[:], in_=x_[:], func=AF.Square, accum_out=ss[i][:]), reads=[x_], writes=[junk, ss[i]])
                    rsqrt(rstd[i], ss[i], 1.0 / D, EPS)
                    dve.op(lambda e: e.scalar_tensor_tensor(out=tmp[i][:], in0=x_[:], scalar=rstd[i][:, 0:1], in1=tabs[:, si, 1, :], op0=ALU.mult, op1=ALU.mult),
                           reads=[x_, rstd[i], tabs], writes=[tmp[i]])
                    pool.op(lambda e: e.tensor_tensor(out=hb[i][:], in0=tmp[i][:], in1=tabs[:, si, 0, :], op=ALU.add), reads=[tmp[i], tabs], writes=[hb[i]])
                    p_ = ptr[c_ % 2]
                    for kc in range(8):
                        pe.op(lambda e: e.transpose(p_[:, kc, :], hb[i][:, kc * 128:(kc + 1) * 128], ident_b[:]), reads=[hb[i], ident_b], writes=[p_])
                    do_copy(act if c_ % 2 == 0 else dve, hTb[:, :, ci * 128:(ci + 1) * 128], p_[:], [p_], [hTb])
                n = ncb * 128
                col0 = seg_of(c0 * 128)
                for kc in range(8):
                    pool.dma(hTd[kc * 128:(kc + 1) * 128, col0:col0 + n], hTb[:, kc, 0:n], reads=[hTb], writes=[D_x])
        fw.barrier()


    def phase_A1(l):
        with ExitStack() as es:
            W = SB(es, "W", [128, 8, NEXT], BF16)
            for kc in range(8):
                sp.dma(W[:, kc, :], winb[l][kc * 128:(kc + 1) * 128, :], writes=[W])
            cw = SB(es, "cw", [128, 6, 5], F32)
            cb = SB(es, "cb", [128, 6], F32)
            with nc.allow_non_contiguous_dma(reason="tiny"):
                for pt_ in range(6):
                    sp.dma(cw[:, pt_, :], conv_w[l][:, pt_ * 128:(pt_ + 1) * 128].rearrange("k p -> p k"), writes=[cw])
                sp.dma(cb[:], conv_b[l].rearrange("(t p) -> p t", p=128), writes=[cb])
            hTb = SB(es, "hTb", [128, 8, 516], BF16)
            rc_ = SB(es, "rc_", [128, 512], F32)
            rs_ = SB(es, "rs_", [128, 512], F32)
            xbc = SB(es, "xbc", [128, 6, 512], BF16)
            cv = [SB(es, "cv%d" % i, [128, 512], F32) for i in range(2)]
            qk = SB(es, "qk", [128, 2, 4, 512], BF16)
            t1 = SB(es, "t1", [128, 512], F32)
            t2 = SB(es, "t2", [128, 512], F32)
            outb = SB(es, "outb", [128, 5120], BF16)
            dtt = SB(es, "dtt", [128, 16], F32)
            xtm = SB(es, "xtm", [128, 640], BF16)
            ktm = SB(es, "ktm", [128, 512], BF16)
            pre = PS(es, "pre", [128, 1024])
            pr = [PS(es, "pr%d" % i, [128, 512]) for i in range(2)]
            ptm = [PS(es, "ptm%d" % i, [128, 512]) for i in range(2)]
            ptr = PS(es, "ptrA", [128, 8, 128], BF16)
            blocks = [(0, 256)] + [(256 + 512 * j, 512) for j in range(16)]
            for (tok0, n) in blocks:
                col0 = seg_of(tok0)
                for kc in range(8):
                    sp.dma(hTb[:, kc, 0:n + 4], hTd[kc * 128:(kc + 1) * 128, col0 - 2:col0 + n + 2], writes=[hTb])
                sp.dma(rc_[:, 0:n], cin["ropec"][:, tok0:tok0 + n], writes=[rc_])
                sp.dma(rs_[:, 0:n], cin["ropes"][:, tok0:tok0 + n], writes=[rs_])
                ntt = n // 128
                for pt in range(6):
                    c0 = C_XBC + pt * 128
                    n1 = min(512, n + 4)
                    for kc in range(8):
                        pe.op(lambda e: e.matmul(pre[:, 0:n1], W[:, kc, c0:c0 + 128], hTb[:, kc, 0:n1], start=(kc == 0), stop=(kc == 7)), reads=[W, hTb], writes=[pre])
                    if n + 4 > 512:
                        for kc in range(8):
                            pe.op(lambda e: e.matmul(pre[:, 512:n + 4], W[:, kc, c0:c0 + 128], hTb[:, kc, 512:n + 4], start=(kc == 0), stop=(kc == 7)), reads=[W, hTb], writes=[pre])
                    c_ = cv[pt % 2]
                    dve.op(lambda e: e.tensor_scalar(out=c_[:, 0:n], in0=pre[:, 0:n], scalar1=cw[:, pt, 0:1], scalar2=None, op0=ALU.mult), reads=[pre, cw], writes=[c_])
                    for k in range(1, 5):
                        dve.op(lambda e: e.scalar_tensor_tensor(out=c_[:, 0:n], in0=pre[:, k:k + n], scalar=cw[:, pt, k:k + 1], in1=c_[:, 0:n], op0=ALU.mult, op1=ALU.add),
                               reads=[pre, cw, c_], writes=[c_])
                    act.op(lambda e: e.activation(out=xbc[:, pt, 0:n], in_=c_[:, 0:n], func=AF.Silu, bias=cb[:, pt:pt + 1], scale=1.0), reads=[c_, cb], writes=[xbc])
                pool.dma(BCt_d[0:128, tok0:tok0 + n], xbc[:, 4, 0:n], reads=[xbc], writes=[D_x])
                pool.dma(BCt_d[128:256, tok0:tok0 + n], xbc[:, 5, 0:n], reads=[xbc], writes=[D_x])
                for tt in range(ntt):
                    for pt in range(5):
                        pe.op(lambda e: e.transpose(ptr[:, pt, :], xbc[:, pt, tt * 128:(tt + 1) * 128], ident_b[:]), reads=[xbc, ident_b], writes=[ptr])
                    dve.op(lambda e: e.tensor_copy(out=xtm[:], in_=ptr[:, 0:5, :]), reads=[ptr], writes=[xtm])
                    r0 = tok0 + tt * 128
                    pool.dma(X_d[r0:r0 + 128, :], xtm[:, 0:512], reads=[xtm], writes=[D_x])
                    pool.dma(Btm_d[r0:r0 + 128, :], xtm[:, 512:640], reads=[xtm], writes=[D_x])
                for wh, (cA, cP, dst) in enumerate(((C_Q, C_QP, Qt_d), (C_K, C_KP, Kt_d))):
                    for hp in range(4):
                        for j, cc in enumerate((cA, cP)):
                            for kc in range(8):
                                pe.op(lambda e: e.matmul(pr[j][:, 0:n], W[:, kc, cc + hp * 128:cc + (hp + 1) * 128], hTb[:, kc, 2:2 + n], start=(kc == 0), stop=(kc == 7)),
                                      reads=[W, hTb], writes=[pr[j]])
                        dve.op(lambda e: e.tensor_tensor(out=t1[:, 0:n], in0=pr[0][:, 0:n], in1=rc_[:, 0:n], op=ALU.mult), reads=[pr[0], rc_], writes=[t1])
                        dve.op(lambda e: e.tensor_tensor(out=t2[:, 0:n], in0=pr[1][:, 0:n], in1=rs_[:, 0:n], op=ALU.mult), reads=[pr[1], rs_], writes=[t2])
                        pool.op(lambda e: e.tensor_tensor(out=qk[:, wh, hp, 0:n], in0=t1[:, 0:n], in1=t2[:, 0:n], op=ALU.add), reads=[t1, t2], writes=[qk])
                        pool.dma(dst[hp * 128:(hp + 1) * 128, tok0:tok0 + n], qk[:, wh, hp, 0:n], reads=[qk], writes=[D_x])
                for tt in range(ntt):
                    for hp in range(4):
                        pe.op(lambda e: e.transpose(ptr[:, hp, :], qk[:, 1, hp, tt * 128:(tt + 1) * 128], ident_b[:]), reads=[qk, ident_b], writes=[ptr])
                    dve.op(lambda e: e.tensor_copy(out=ktm[:], in_=ptr[:, 0:4, :]), reads=[ptr], writes=[ktm])
                    r0 = tok0 + tt * 128
                    pool.dma(Ktm_d[r0:r0 + 128, :], ktm[:], reads=[ktm], writes=[D_x])
                for tt in range(ntt):
                    r0 = tok0 + tt * 128
                    lT = lambda kc: hTb[:, kc, 2 + tt * 128:2 + (tt + 1) * 128]
                    specs = [(C_Z, 0, "silu"), (C_POOL, 512, "copy"), (C_V, 1024, "copy"), (C_G, 1536, "silu")] + [(C_GATE + 512 * j, 2048 + 512 * j, "sig") for j in range(6)]
                    for si_, (cc, oc, kind) in enumerate(specs):
                        p_ = ptm[si_ % 2]
                        for kc in range(8):
                            pe.op(lambda e: e.matmul(p_[:], lT(kc), W[:, kc, cc:cc + 512], start=(kc == 0), stop=(kc == 7)), reads=[hTb, W], writes=[p_])
                        if kind == "copy":
                            dve.op(lambda e: e.tensor_copy(out=outb[:, oc:oc + 512], in_=p_[:]), reads=[p_], writes=[outb])
                        else:
                            fn = AF.Silu if kind == "silu" else AF.Sigmoid
                            act.op(lambda e: e.activation(out=outb[:, oc:oc + 512], in_=p_[:], func=fn), reads=[p_], writes=[outb])
                    p_ = ptm[0]
                    for kc in range(8):
                        pe.op(lambda e: e.matmul(p_[:, 0:16], lT(kc), W[:, kc, C_DT:C_DT + 16], start=(kc == 0), stop=(kc == 7)), reads=[hTb, W], writes=[p_])
                    dve.op(lambda e: e.tensor_copy(out=dtt[:], in_=p_[:, 0:16]), reads=[p_], writes=[dtt])
                    pool.dma(zs_d[r0:r0 + 128, :], outb[:, 0:512], reads=[outb], writes=[D_x])
                    pool.dma(up_d[r0:r0 + 128, :], outb[:, 512:1024], reads=[outb], writes=[D_x])
                    pool.dma(vv_d[r0:r0 + 128, :], outb[:, 1024:1536], reads=[outb], writes=[D_x])
                    pool.dma(gs_d[r0:r0 + 128, :], outb[:, 1536:2048], reads=[outb], writes=[D_x])
                    pool.dma(gt_d[r0:r0 + 128, :], outb[:, 2048:5120], reads=[outb], writes=[D_x])
                    pool.dma(dtr_d[r0:r0 + 128, :], dtt[:], reads=[dtt], writes=[D_x])
        fw.barrier()


    brs_d = dscr("brs_d", [S, 1536], BF16)

    def bc_last(ap, n):
        sh = list(ap.shape)
        return ap.unsqueeze(len(sh)).to_broadcast(sh + [n])

    def layer_tabs(es, l):
        T = {}
        raw = SB(es, "raw16", [128, 3, 16], F32)
        sp.dma(raw[:, 0, :], dt_bias[l:l + 1, :].partition_broadcast(128), writes=[raw])
        sp.dma(raw[:, 1, :], a_log[l:l + 1, :].partition_broadcast(128), writes=[raw])
        sp.dma(raw[:, 2, :], ret_dl[l:l + 1, :].partition_broadcast(128), writes=[raw])
        nega = SB(es, "nega", [128, 16], F32)
        act.op(lambda e: e.activation(out=nega[:], in_=raw[:, 1, :], func=AF.Exp), reads=[raw], writes=[nega])
        dve.op(lambda e: e.tensor_scalar(out=nega[:], in0=nega[:], scalar1=-1.0, scalar2=None, op0=ALU.mult), reads=[nega], writes=[nega])
        lg = SB(es, "lg", [128, 16], F32)
        act.op(lambda e: e.activation(out=lg[:], in_=raw[:, 2, :], func=AF.Exp, scale=-1.0), reads=[raw], writes=[lg])
        act.op(lambda e: e.activation(out=lg[:], in_=lg[:], func=AF.Ln, bias=1.0, scale=1.0), reads=[lg], writes=[lg])
        dve.op(lambda e: e.tensor_scalar(out=lg[:], in0=lg[:], scalar1=-1.0, scalar2=None, op0=ALU.mult), reads=[lg], writes=[lg])
        posb = SB(es, "posb", [128, 3], F32)
        sp.dma(posb[:], cin["pos"][:, :], writes=[posb])
        T.update(raw=raw, nega=nega, lg=lg, posb=posb)
        return T

    def phase_B1(l):
        with ExitStack() as es:
            T = layer_tabs(es, l)
            raw, nega, lg, posb = T["raw"], T["nega"], T["lg"], T["posb"]
            kdec = SB(es, "kdec", [128, 2, 8], F32)
            for d in range(2):
                act.op(lambda e: e.activation(out=kdec[:, d, :], in_=lg[:, d * 8:(d + 1) * 8], func=AF.Exp, scale=posb[:, d:d + 1]), reads=[lg, posb], writes=[kdec])
            triu = SB(es, "triu", [128, 128], F32)
            ones = SB(es, "ones", [128, 128], F32)
            sp.dma(triu[:], cin["triu"][:, :], writes=[triu])
            sp.dma(ones[:], cin["ones"][:, :], writes=[ones])
            NB = 2
            Xb = [SB(es, "Xb%d" % i, [128, 512], BF16) for i in range(NB)]
            Bb = [SB(es, "Bb%d" % i, [128, 128], BF16) for i in range(NB)]
            dtr = [SB(es, "dtr%d" % i, [128, 16], F32) for i in range(NB)]
            Kb = [SB(es, "Kb%d" % i, [128, 512], BF16) for i in range(NB)]
            Vb = [SB(es, "Vb%d" % i, [128, 512], BF16) for i in range(NB)]
            dtv = SB(es, "dtv", [128, 16], F32)
            cs2 = [SB(es, "cs2%d" % i, [128, 48], F32) for i in range(NB)]
            t16 = SB(es, "t16", [128, 16], F32)
            dsx = SB(es, "dsx", [128, 16], F32)
            cd = [SB(es, "cd%d" % i, [64, 16], F32) for i in range(NB)]
            Xdt = [SB(es, "Xdt%d" % i, [128, 2, 512], BF16) for i in range(NB)]
            Bdec = SB(es, "Bdec", [128, 2, 8, 64], BF16)
            Vdec = SB(es, "Vdec", [128, 2, 512], BF16)
            stt = [SB(es, "stt%d" % i, [64, 4, 512], F32) for i in range(NB)]
            pc = PS(es, "pc", [128, 512])
            pst = PS(es, "pst", [64, 2, 512])
            pkv = PS(es, "pkv", [64, 2, 512])
            for c in range(NCH):
                i = c % NB
                r0 = c * 128
                sp.dma(Xb[i][:], X_d[r0:r0 + 128, :], writes=[Xb[i]])
                sp.dma(Bb[i][:], Btm_d[r0:r0 + 128, :], writes=[Bb[i]])
                sp.dma(dtr[i][:], dtr_d[r0:r0 + 128, :], writes=[dtr[i]])
                sp.dma(Kb[i][:], Ktm_d[r0:r0 + 128, :], writes=[Kb[i]])
                sp.dma(Vb[i][:], vv_d[r0:r0 + 128, :], writes=[Vb[i]])
                c2 = cs2[i]
                dve.op(lambda e: e.tensor_tensor(out=dtv[:], in0=dtr[i][:], in1=raw[:, 0, :], op=ALU.add), reads=[dtr[i], raw], writes=[dtv])
                act.op(lambda e: e.activation(out=dtv[:], in_=dtv[:], func=AF.Exp), reads=[dtv], writes=[dtv])
                act.op(lambda e: e.activation(out=dtv[:], in_=dtv[:], func=AF.Ln, bias=1.0, scale=1.0), reads=[dtv], writes=[dtv])
                dve.op(lambda e: e.tensor_tensor(out=c2[:, 32:48], in0=dtv[:], in1=nega[:], op=ALU.mult), reads=[dtv, nega], writes=[c2])
                pe.op(lambda e: e.matmul(pc[:, 0:16], triu[:], c2[:, 32:48], start=True, stop=True), reads=[triu, c2], writes=[pc])
                pe.op(lambda e: e.matmul(pc[:, 16:32], ones[:], c2[:, 32:48], start=True, stop=True), reads=[ones, c2], writes=[pc])
                dve.op(lambda e: e.tensor_copy(out=c2[:, 0:32], in_=pc[:, 0:32]), reads=[pc], writes=[c2])
                dve.op(lambda e: e.tensor_tensor(out=c2[:, 8:16], in0=c2[:, 8:16], in1=c2[:, 40:48], op=ALU.subtract), reads=[c2], writes=[c2])
                dve.op(lambda e: e.tensor_tensor(out=t16[:, 0:8], in0=c2[:, 16:24], in1=c2[:, 0:8], op=ALU.subtract), reads=[c2], writes=[t16])
                dve.op(lambda e: e.tensor_copy(out=t16[:, 8:16], in_=c2[:, 8:16]), reads=[c2], writes=[t16])
                act.op(lambda e: e.activation(out=dsx[:], in_=t16[:], func=AF.Exp), reads=[t16], writes=[dsx])
                act.op(lambda e: e.activation(out=cd[i][:], in_=c2[0:64, 16:32], func=AF.Exp), reads=[c2], writes=[cd[i]])
                pool.dma(cdec_d[c], cd[i][:], reads=[cd[i]], writes=[D_x])
                pool.dma(cs2_d[r0:r0 + 128, :], c2[:], reads=[c2], writes=[D_x])
                xd = Xdt[i]
                for d in range(2):
                    dve.op(lambda e: e.tensor_tensor(out=xd[:, d, :].rearrange("p (h q) -> p h q", h=8), in0=Xb[i][:].rearrange("p (h q) -> p h q", h=8),
                                                     in1=bc_last(dtv[:, d * 8:(d + 1) * 8], 64), op=ALU.mult), reads=[Xb[i], dtv], writes=[xd])
                pool.dma(Xdt_d[r0:r0 + 128], xd[:], reads=[xd], writes=[D_x])
                for d in range(2):
                    bview = Bb[i][:].rearrange("p (g n) -> p g n", g=2).unsqueeze(2).to_broadcast([128, 2, 4, 64])
                    dview = bc_last(dsx[:, d * 8:(d + 1) * 8].rearrange("p (g r) -> p g r", g=2), 64)
                    dve.op(lambda e: e.tensor_tensor(out=Bdec[:, d].rearrange("p (g r) n -> p g r n", g=2), in0=bview, in1=dview, op=ALU.mult), reads=[Bb[i], dsx], writes=[Bdec])
                    dve.op(lambda e: e.tensor_tensor(out=Vdec[:, d, :].rearrange("p (h q) -> p h q", h=8), in0=Vb[i][:].rearrange("p (h q) -> p h q", h=8),
                                                     in1=bc_last(kdec[:, d, :], 64), op=ALU.mult), reads=[Vb[i], kdec], writes=[Vdec])
                for d in range(2):
                    for h in range(8):
                        pe.op(lambda e: e.matmul(pst[:, d, h * 64:(h + 1) * 64], Bdec[:, d, h, :], xd[:, d, h * 64:(h + 1) * 64], start=True, stop=True), reads=[Bdec, xd], writes=[pst])
                        pe.op(lambda e: e.matmul(pkv[:, d, h * 64:(h + 1) * 64], Kb[i][:, h * 64:(h + 1) * 64], Vdec[:, d, h * 64:(h + 1) * 64], start=True, stop=True), reads=[Kb[i], Vdec], writes=[pkv])
                st_ = stt[i]
                dve.op(lambda e: e.tensor_copy(out=st_[:, 0:2, :], in_=pst[:]), reads=[pst], writes=[st_])
                act.op(lambda e: e.copy(out=st_[:, 2:4, :], in_=pkv[:]), reads=[pkv], writes=[st_])
                pool.dma(st_d[c], st_[:], reads=[st_], writes=[D_x])
        fw.barrier()

    def phase_B2(l):
        with ExitStack() as es:
            T = layer_tabs(es, l)
            lg, posb = T["lg"], T["posb"]
            rdec = SB(es, "rdec", [64, 16], F32)
            act.op(lambda e: e.activation(out=rdec[:], in_=lg[0:64, :], func=AF.Exp, scale=posb[0:64, 2:3]), reads=[lg, posb], writes=[rdec])
            ST = SB(es, "ST", [64, 4, 512], F32)
            dve.op(lambda e: e.memset(ST[:], 0.0), writes=[ST])
            NB = 3
            IN = [SB(es, "IN%d" % i, [64, 4, 512], F32) for i in range(NB)]
            DC = [SB(es, "DC%d" % i, [64, 16], F32) for i in range(NB)]
            PV = [SB(es, "PV%d" % i, [64, 4, 512], BF16) for i in range(NB)]
            order_f = list(range(NCH))
            order_b = [1, 0] + list(range(NCH - 1, 1, -1))
            for k in range(NCH):
                i = k % NB
                cf, cbk = order_f[k], order_b[k]
                sp.dma(IN[i][:, 0, :], st_d[cf, :, 0, :], writes=[IN[i]])
                sp.dma(IN[i][:, 1, :], st_d[cbk, :, 1, :], writes=[IN[i]])
                sp.dma(IN[i][:, 2, :], st_d[cf, :, 2, :], writes=[IN[i]])
                sp.dma(IN[i][:, 3, :], st_d[cbk, :, 3, :], writes=[IN[i]])
                sp.dma(DC[i][:, 0:8], cdec_d[cf, :, 0:8], writes=[DC[i]])
                sp.dma(DC[i][:, 8:16], cdec_d[cbk, :, 8:16], writes=[DC[i]])
                act.op(lambda e: e.copy(out=PV[i][:], in_=ST[:]), reads=[ST], writes=[PV[i]])
                pool.dma(prev_d[cf, :, 0, :], PV[i][:, 0, :], reads=[PV[i]], writes=[D_x])
                pool.dma(prev_d[cbk, :, 1, :], PV[i][:, 1, :], reads=[PV[i]], writes=[D_x])
                pool.dma(prev_d[cf, :, 2, :], PV[i][:, 2, :], reads=[PV[i]], writes=[D_x])
                pool.dma(prev_d[cbk, :, 3, :], PV[i][:, 3, :], reads=[PV[i]], writes=[D_x])
                dve.op(lambda e: e.tensor_tensor(out=ST[:, 0:2, :].rearrange("p d (h q) -> p (d h) q", h=8), in0=ST[:, 0:2, :].rearrange("p d (h q) -> p (d h) q", h=8),
                                                 in1=bc_last(DC[i][:], 64), op=ALU.mult), reads=[ST, DC[i]], writes=[ST])
                dve.op(lambda e: e.tensor_tensor(out=ST[:, 2:4, :].rearrange("p d (h q) -> p (d h) q", h=8), in0=ST[:, 2:4, :].rearrange("p d (h q) -> p (d h) q", h=8),
                                                 in1=bc_last(rdec[:], 64), op=ALU.mult), reads=[ST, rdec], writes=[ST])
                dve.op(lambda e: e.tensor_tensor(out=ST[:], in0=ST[:], in1=IN[i][:], op=ALU.add), reads=[ST, IN[i]], writes=[ST])
        fw.barrier()


    def phase_C(l, need_ctx=True):
        with ExitStack() as es:
            T = layer_tabs(es, l)
            lg = T["lg"]
            wbr = SB(es, "wbr", [128, 12, D], BF16)
            wo = SB(es, "wo", [128, 8, D], BF16)
            sp.dma(wbr[:], wbrb[l].rearrange("(kc k) c -> k kc c", k=128), writes=[wbr])
            sp.dma(wo[:], woutb[l].rearrange("(kc k) c -> k kc c", k=128), writes=[wo])
            pwf = SB(es, "pwf", [128, 4, 128], F32)
            pw = SB(es, "pw", [128, 4, 128], BF16)
            sp.dma(pwf[:], pool_w[l].rearrange("g c o -> c g o"), writes=[pwf])
            dve.op(lambda e: e.tensor_copy(out=pw[:], in_=pwf[:]), reads=[pwf], writes=[pw])
            pPf = SB(es, "pPf", [128, 20, 128], F32)
            pP = SB(es, "pP", [128, 4, 5, 128], BF16)
            sp.dma(pPf[:], cin["poolP"].rearrange("t g k u -> t (g k) u"), writes=[pPf])
            dve.op(lambda e: e.tensor_copy(out=pP[:].rearrange("t g k u -> t (g k) u"), in_=pPf[:]), reads=[pPf], writes=[pP])
            rdf = SB(es, "rdf", [128, 5, 128], F32)
            sp.dma(rdf[:], cin["rdiff"][:, :, :], writes=[rdf])
            prow = SB(es, "prow", [128, 2, 128], F32)
            sp.dma(prow[:], cin["prow"][:, :, :], writes=[prow])
            mfb = SB(es, "mfb", [128, 2, 512], F32)
            sp.dma(mfb[:], cin["maskfb"][:, :, :], writes=[mfb])
            eall = SB(es, "eall", [16, 2, 8, 128], F32)
            sp.dma(eall[:], cin["eall"][:, :, :, :], writes=[eall])
            ones16 = SB(es, "ones16", [16, 128], F32)
            dve.op(lambda e: e.memset(ones16[:], 1.0), writes=[ones16])
            g1t = SB(es, "g1t", [128, 2, D], F32)
            for s_ in range(2):
                sp.dma(g1t[:, s_, :], modtab[l, s_, 2], writes=[g1t])
            v8 = SB(es, "v8", [128, 8], F32)
            sp.dma(v8[:], ssd_d[l:l + 1, :].partition_broadcast(128), writes=[v8])
            Dtab = SB(es, "Dtab", [128, 8, 64], F32)
            dve.op(lambda e: e.tensor_copy(out=Dtab[:], in_=bc_last(v8[:], 64)), reads=[v8], writes=[Dtab])
            ngt = SB(es, "ngt", [128, 512], F32)
            sp.dma(ngt[:], ssd_ng[l:l + 1, :].partition_broadcast(128), writes=[ngt])
            psct = SB(es, "psct", [128, 512], F32)
            sp.dma(psct[:], pool_sc[l:l + 1, :].partition_broadcast(128), writes=[psct])
            dcomb = SB(es, "dcomb", [128, 8, 128], F32)
            tA = SB(es, "tA", [128, 128], F32)
            tB = SB(es, "tB", [128, 128], F32)
            for h in range(8):
                act.op(lambda e: e.activation(out=tA[:], in_=rdf[:, 0, :], func=AF.Exp, scale=lg[:, h:h + 1]), reads=[rdf, lg], writes=[tA])
                act.op(lambda e: e.activation(out=tB[:], in_=rdf[:, 1, :], func=AF.Exp, scale=lg[:, 8 + h:9 + h]), reads=[rdf, lg], writes=[tB])
                dve.op(lambda e: e.tensor_tensor(out=tA[:], in0=tA[:], in1=rdf[:, 2, :], op=ALU.mult), reads=[tA, rdf], writes=[tA])
                dve.op(lambda e: e.tensor_tensor(out=tB[:], in0=tB[:], in1=rdf[:, 3, :], op=ALU.mult), reads=[tB, rdf], writes=[tB])
                dve.op(lambda e: e.tensor_tensor(out=tA[:], in0=tA[:], in1=tB[:], op=ALU.add), reads=[tA, tB], writes=[tA])
                dve.op(lambda e: e.tensor_tensor(out=dcomb[:, h, :], in0=tA[:], in1=rdf[:, 4, :], op=ALU.add), reads=[tA, rdf], writes=[dcomb])
            qdec = SB(es, "qdec", [128, 2, 4, 128], F32)
            for d in range(2):
                for hp in range(4):
                    for hh in range(2):
                        h = 2 * hp + hh
                        act.op(lambda e: e.activation(out=qdec[hh * 64:(hh + 1) * 64, d, hp, :], in_=prow[hh * 64:(hh + 1) * 64, d, :], func=AF.Exp,
                                                      scale=lg[hh * 64:(hh + 1) * 64, d * 8 + h:d * 8 + h + 1]), reads=[prow, lg], writes=[qdec])
            Bt2 = [SB(es, "Bt", [128, 128], BF16) for _ in range(2)]
            Ctz2 = [SB(es, "Ctz", [128, 2, 128], BF16) for _ in range(2)]
            for t_ in Ctz2:
                dve.op(lambda e: e.memset(t_[:], 0.0), writes=[t_])
            xd2 = [SB(es, "xdC", [128, 2, 512], BF16) for _ in range(2)]
            c22 = [SB(es, "c2C", [128, 48], F32) for _ in range(2)]
            prevb2 = [SB(es, "prevb", [128, 4, 512], BF16) for _ in range(2)]
            Qz2 = [SB(es, "Qz", [128, 2, 4, 128], BF16) for _ in range(2)]
            Kz2 = [SB(es, "Kz", [128, 2, 4, 128], BF16) for _ in range(2)]
            for t_ in Qz2 + Kz2:
                dve.op(lambda e: e.memset(t_[:], 0.0), writes=[t_])
            Vc2 = [SB(es, "Vc", [128, 512], BF16) for _ in range(2)]
            zsb2 = [SB(es, "zsb", [128, 512], BF16) for _ in range(2)]
            gsb2 = [SB(es, "gsb", [128, 512], BF16) for _ in range(2)]
            gtb2 = [SB(es, "gtb", [128, 3072], BF16) for _ in range(2)]
            upb2 = [[SB(es, "upb%d" % i, [128, 512], BF16) for i in range(3)] for _ in range(2)]
            Xc2 = [SB(es, "Xc", [128, 512], BF16) for _ in range(2)]
            xr2 = [SB(es, "xr", [128, D], F32) for _ in range(2)]
            nacs = SB(es, "nacs", [128, 16], F32)
            rowsT = SB(es, "rowsT", [16, 2, 128], F32)
            RD = SB(es, "RD", [16, 2, 8, 128], F32)
            Lm = SB(es, "Lm", [128, 8, 128], BF16)
            Mm = SB(es, "Mm", [128, 2, 8, 128], BF16)
            XD = SB(es, "XD", [128, 512], BF16)
            E16 = SB(es, "E16", [128, 16], F32)
            y1 = SB(es, "y1", [128, 512], F32)
            y2 = SB(es, "y2", [128, 512], F32)
            st8 = SB(es, "st8", [128, 4, 8], F32)
            s1 = SB(es, "s1", [128, 1], F32)
            junk = SB(es, "junkC", [128, 512], F32)
            brs = SB(es, "brs", [128, 1536], BF16)
            brT = SB(es, "brT", [128, 12, 128], BF16)
            Qs = SB(es, "Qs", [128, 2, 2, 4, 128], BF16)
            IM = SB(es, "IM", [128, 8, 128], BF16)
            mixT = SB(es, "mixT", [128, 4, 128], BF16)
            acc = SB(es, "acc", [128, D], F32)
            tmpm = SB(es, "tmpm", [128, D], F32)
            accb = SB(es, "accb", [128, D], BF16)
            accT = SB(es, "accT", [128, 8, 128], BF16)
            p2 = [PS(es, "p2%d" % i, [128, 1024]) for i in range(2)]
            p1 = [PS(es, "p1%d" % i, [128, 512]) for i in range(4)]

            def segs_of(c):
                return (0, 2) if c < 2 else (2, NCH)

            for c in (range(0 if need_ctx else 2, NCH) if cstage is None else range(3)):
                r0 = c * 128
                lo, hi = segs_of(c)
                pb_ = c % 2
                Bt, Ctz, xd, c2, prevb, Qz, Kz, Vc, zsb, gsb, gtb, upb, Xc, xr = (Bt2[pb_], Ctz2[pb_], xd2[pb_], c22[pb_], prevb2[pb_], Qz2[pb_], Kz2[pb_],
                                                                                   Vc2[pb_], zsb2[pb_], gsb2[pb_], gtb2[pb_], upb2[pb_], Xc2[pb_], xr2[pb_])
                si = 0 if c < 2 else 1
                sp.dma(Bt[:], BCt_d[0:128, r0:r0 + 128], writes=[Bt])
                sp.dma(Ctz[0:64, 0, :], BCt_d[128:192, r0:r0 + 128], writes=[Ctz])
                sp.dma(Ctz[64:128, 1, :], BCt_d[192:256, r0:r0 + 128], writes=[Ctz])
                sp.dma(xd[:], Xdt_d[r0:r0 + 128], writes=[xd])
                sp.dma(c2[:], cs2_d[r0:r0 + 128, :], writes=[c2])
                sp.dma(prevb[0:64], prev_d[c], writes=[prevb])
                sp.dma(prevb[64:128], prev_d[c], writes=[prevb])
                for hh_ in range(2):
                    ps__ = slice(hh_ * 64, (hh_ + 1) * 64)
                    sp.dma(Qz[ps__, hh_], Qt_d[:, r0:r0 + 128].rearrange("(hp p) t -> p hp t", p=128)[ps__], writes=[Qz])
                    sp.dma(Kz[ps__, hh_], Kt_d[:, r0:r0 + 128].rearrange("(hp p) t -> p hp t", p=128)[ps__], writes=[Kz])
                sp.dma(Vc[:], vv_d[r0:r0 + 128, :], writes=[Vc])
                sp.dma(zsb[:], zs_d[r0:r0 + 128, :], writes=[zsb])
                sp.dma(gsb[:], gs_d[r0:r0 + 128, :], writes=[gsb])
                sp.dma(gtb[:], gt_d[r0:r0 + 128, :], writes=[gtb])
                sp.dma(Xc[:], X_d[r0:r0 + 128, :], writes=[Xc])
                sp.dma(xr[:], xres[r0:r0 + 128, :], writes=[xr])
                ups = {}
                for dc in (-1, 0, 1):
                    cc = c + dc
                    if lo <= cc < hi:
                        ub = upb[dc + 1]
                        sp.dma(ub[:], up_d[cc * 128:(cc + 1) * 128, :], writes=[ub])
                        ups[dc] = ub
                if cstage == 0:
                    continue
                pe.op(lambda e: e.transpose(p1[0][0:16, 0:128], c2[:, 0:16], ident_f[:]), reads=[c2, ident_f], writes=[p1[0]])
                dve.op(lambda e: e.tensor_copy(out=rowsT[:, 0, :], in_=p1[0][0:16, 0:128]), reads=[p1[0]], writes=[rowsT])
                dve.op(lambda e: e.tensor_scalar(out=rowsT[:, 1, :], in0=rowsT[:, 0, :], scalar1=-1.0, scalar2=None, op0=ALU.mult), reads=[rowsT], writes=[rowsT])
                dve.op(lambda e: e.tensor_tensor(out=RD[:, 0], in0=eall[:, 0], in1=rowsT[:, 0:1, :].to_broadcast([16, 8, 128]), op=ALU.mult), reads=[eall, rowsT], writes=[RD])
                dve.op(lambda e: e.tensor_tensor(out=RD[:, 1], in0=eall[:, 1], in1=rowsT[:, 1:2, :].to_broadcast([16, 8, 128]), op=ALU.mult), reads=[eall, rowsT], writes=[RD])
                if cstage == 1:
                    continue
                for g in range(2):
                    pe.op(lambda e: e.matmul(p1[1][:, g * 128:(g + 1) * 128], Bt[:], Ctz[:, g, :], start=True, stop=True), reads=[Bt, Ctz], writes=[p1[1]])
                for d in range(2):
                    seg = p2[d]
                    for hf in range(2):
                        o_ = seg[:, hf * 512:(hf + 1) * 512]
                        rd_ = RD[:, d, hf * 4:(hf + 1) * 4, :].rearrange("k h l -> k (h l)")
                        ea_ = eall[:, d, hf * 4:(hf + 1) * 4, :].rearrange("k h l -> k (h l)")
                        pe.op(lambda e: e.matmul(o_, ones16[:], rd_, start=True, stop=False), reads=[ones16, RD], writes=[seg])
                        pe.op(lambda e: e.matmul(o_, rowsT[:, 1 - d, :], ea_, start=False, stop=False), reads=[rowsT, eall], writes=[seg])
                        pe.op(lambda e: e.matmul(o_, ident_f[:], mfb[:, d, :], start=False, stop=True), reads=[ident_f, mfb], writes=[seg])
                    act.op(lambda e: e.activation(out=Lm[:].rearrange("p h l -> p (h l)"), in_=seg[:], func=AF.Exp), reads=[seg], writes=[Lm])
                    scv = p1[1][:, 0:256].rearrange("p (g l) -> p g l", g=2).unsqueeze(2).to_broadcast([128, 2, 4, 128])
                    dve.op(lambda e: e.tensor_tensor(out=Mm[:, d].rearrange("p (g r) l -> p g r l", g=2), in0=Lm[:].rearrange("p (g r) l -> p g r l", g=2), in1=scv, op=ALU.mult),
                           reads=[Lm, p1[1]], writes=[Mm])
                if cstage == 2:
                    continue
                pool.op(lambda e: e.tensor_tensor(out=XD[:].rearrange("p (h q) -> p h q", h=8), in0=Xc[:].rearrange("p (h q) -> p h q", h=8), in1=Dtab[:], op=ALU.mult), reads=[Xc, Dtab], writes=[XD])
                yd = p1[2]
                for h in range(8):
                    hs = slice(h * 64, (h + 1) * 64)
                    pe.op(lambda e: e.matmul(yd[:, hs], Mm[:, 0, h, :], xd[:, 0, hs], start=True, stop=False), reads=[Mm, xd], writes=[yd])
                    pe.op(lambda e: e.matmul(yd[:, hs], Mm[:, 1, h, :], xd[:, 1, hs], start=False, stop=False), reads=[Mm, xd], writes=[yd])
                    pe.op(lambda e: e.matmul(yd[:, hs], ident_b[:], XD[:, hs], start=False, stop=True), reads=[ident_b, XD], writes=[yd])
                yo = p2[0]
                for d in range(2):
                    for h in range(8):
                        g = h // 4
                        pe.op(lambda e: e.matmul(yo[:, d * 512 + h * 64:d * 512 + (h + 1) * 64], Ctz[:, g, :], prevb[:, d, h * 64:(h + 1) * 64], start=True, stop=True),
                              reads=[Ctz, prevb], writes=[yo])
                dve.op(lambda e: e.tensor_copy(out=nacs[:, 0:8], in_=c2[:, 0:8]), reads=[c2], writes=[nacs])
                dve.op(lambda e: e.tensor_tensor(out=nacs[:, 8:16], in0=c2[:, 24:32], in1=c2[:, 8:16], op=ALU.subtract), reads=[c2], writes=[nacs])
                act.op(lambda e: e.activation(out=E16[:], in_=nacs[:], func=AF.Exp), reads=[nacs], writes=[E16])
                dve.op(lambda e: e.tensor_tensor(out=y1[:].rearrange("p (h q) -> p h q", h=8), in0=yo[:, 0:512].rearrange("p (h q) -> p h q", h=8), in1=bc_last(E16[:, 0:8], 64), op=ALU.mult), reads=[yo, E16], writes=[y1])
                dve.op(lambda e: e.tensor_tensor(out=y2[:].rearrange("p (h q) -> p h q", h=8), in0=yo[:, 512:1024].rearrange("p (h q) -> p h q", h=8), in1=bc_last(E16[:, 8:16], 64), op=ALU.mult), reads=[yo, E16], writes=[y2])
                pool.op(lambda e: e.tensor_tensor(out=y1[:], in0=y1[:], in1=y2[:], op=ALU.add), reads=[y1, y2], writes=[y1])
                dve.op(lambda e: e.tensor_tensor(out=y1[:], in0=y1[:], in1=yd[:], op=ALU.add), reads=[y1, yd], writes=[y1])
                pool.op(lambda e: e.tensor_tensor(out=y1[:], in0=y1[:], in1=zsb[:], op=ALU.mult), reads=[y1, zsb], writes=[y1])
                act.op(lambda e: e.activation(out=junk[:], in_=y1[:], func=AF.Square, accum_out=s1[:]), reads=[y1], writes=[junk, s1])
                rsqrt(s1, s1, 1.0 / 512, EPS)
                dve.op(lambda e: e.scalar_tensor_tensor(out=brs[:, 0:512], in0=y1[:], scalar=s1[:, 0:1], in1=ngt[:], op0=ALU.mult, op1=ALU.mult), reads=[y1, s1, ngt], writes=[brs])
                if cstage == 3:
                    continue
                pm_ = p1[3]
                for gi in range(4):
                    gs_ = slice(gi * 128, (gi + 1) * 128)
                    first = (c == lo)
                    last = (c == hi - 1)
                    kcur = 2 if first else (3 if last else 1)
                    terms = [(ups[0], kcur)]
                    if -1 in ups:
                        terms.append((ups[-1], 0))
                    if 1 in ups:
                        terms.append((ups[1], 4))
                    for ti, (ub, kind) in enumerate(terms):
                        pe.op(lambda e: e.matmul(pm_[:, gs_], ub[:, gs_], pP[:, gi, kind, :], start=(ti == 0), stop=(ti == len(terms) - 1)), reads=[ub, pP], writes=[pm_])
                act.op(lambda e: e.copy(out=mixT[:].rearrange("p g t -> p (g t)"), in_=pm_[:]), reads=[pm_], writes=[mixT])
                po_ = p1[0]
                for gi in range(4):
                    pe.op(lambda e: e.matmul(po_[:, gi * 128:(gi + 1) * 128], mixT[:, gi, :], pw[:, gi, :], start=True, stop=True), reads=[mixT, pw], writes=[po_])
                dve.op(lambda e: e.tensor_tensor(out=brs[:, 512:1024], in0=po_[:], in1=psct[:], op=ALU.mult), reads=[po_, psct], writes=[brs])
                if cstage == 4:
                    continue
                for d in range(2):
                    pool.op(lambda e: e.tensor_tensor(out=Qs[:, d].rearrange("p a h t -> p a (h t)"), in0=Qz[:].rearrange("p a h t -> p a (h t)"),
                                                      in1=qdec[:, d].rearrange("p h t -> p (h t)").unsqueeze(1).to_broadcast([128, 2, 512]), op=ALU.mult), reads=[Qz, qdec], writes=[Qs])
                if cstage == 41:
                    continue
                kq = p2[1]
                for h in range(8):
                    hp, hh = h // 2, h % 2
                    pe.op(lambda e: e.matmul(kq[:, h * 128:(h + 1) * 128], Kz[:, hh, hp, :], Qz[:, hh, hp, :], start=True, stop=True), reads=[Kz, Qz], writes=[kq])
                dve.op(lambda e: e.tensor_tensor(out=IM[:].rearrange("p h l -> p (h l)"), in0=kq[:], in1=dcomb[:].rearrange("p h l -> p (h l)"), op=ALU.mult), reads=[kq, dcomb], writes=[IM])
                if cstage == 42:
                    continue
                yr = p1[1]
                for h in range(8):
                    hp, hh = h // 2, h % 2
                    hs = slice(h * 64, (h + 1) * 64)
                    ps_ = slice(hh * 64, (hh + 1) * 64)
                    pe.op(lambda e: e.matmul(yr[:, hs], IM[:, h, :], Vc[:, hs], start=True, stop=False), reads=[IM, Vc], writes=[yr])
                    pe.op(lambda e: e.matmul(yr[:, hs], Qs[:, 0, hh, hp, :], prevb[:, 2, hs], start=False, stop=False), reads=[Qs, prevb], writes=[yr])
                    pe.op(lambda e: e.matmul(yr[:, hs], Qs[:, 1, hh, hp, :], prevb[:, 3, hs], start=False, stop=True), reads=[Qs, prevb], writes=[yr])
                if cstage == 43:
                    continue
                for h in range(8):
                    hs = slice(h * 64, (h + 1) * 64)
                    act.op(lambda e: e.activation(out=junk[:, hs], in_=yr[:, hs], func=AF.Identity, accum_out=st8[:, 0, h:h + 1]), reads=[yr], writes=[junk, st8])
                    act.op(lambda e: e.activation(out=junk[:, hs], in_=yr[:, hs], func=AF.Square, accum_out=st8[:, 1, h:h + 1]), reads=[yr], writes=[junk, st8])
                dve.op(lambda e: e.tensor_scalar(out=st8[:, 0, :], in0=st8[:, 0, :], scalar1=1.0 / 64, scalar2=None, op0=ALU.mult), reads=[st8], writes=[st8])
                dve.op(lambda e: e.tensor_tensor(out=st8[:, 2, :], in0=st8[:, 0, :], in1=st8[:, 0, :], op=ALU.mult), reads=[st8], writes=[st8])
                dve.op(lambda e: e.scalar_tensor_tensor(out=st8[:, 3, :], in0=st8[:, 1, :], scalar=1.0 / 64, in1=st8[:, 2, :], op0=ALU.mult, op1=ALU.subtract), reads=[st8], writes=[st8])
                act.op(lambda e: e.activation(out=st8[:, 3, :], in_=st8[:, 3, :], func=AF.Sqrt, bias=64.0 * EPS, scale=1.0), reads=[st8], writes=[st8])
                dve.op(lambda e: e.reciprocal(out=st8[:, 3, :], in_=st8[:, 3, :]), reads=[st8], writes=[st8])
                if cstage == 44:
                    continue
                dve.op(lambda e: e.tensor_tensor(out=y2[:].rearrange("p (h q) -> p h q", h=8), in0=yr[:].rearrange("p (h q) -> p h q", h=8), in1=bc_last(st8[:, 0, :], 64), op=ALU.subtract), reads=[yr, st8], writes=[y2])
                dve.op(lambda e: e.tensor_tensor(out=y2[:].rearrange("p (h q) -> p h q", h=8), in0=y2[:].rearrange("p (h q) -> p h q", h=8), in1=bc_last(st8[:, 3, :], 64), op=ALU.mult), reads=[y2, st8], writes=[y2])
                pool.op(lambda e: e.tensor_tensor(out=brs[:, 1024:1536], in0=y2[:], in1=gsb[:], op=ALU.mult), reads=[y2, gsb], writes=[brs])
                if "brs_d" in dbg:
                    pool.dma(brs_d[r0:r0 + 128, :], brs[:], reads=[brs], writes=[D_x])
                if cstage == 5:
                    continue
                trp = p2[1][:].bitcast(BF16).rearrange("p (j t) -> p j t", t=128)
                for j in range(12):
                    pe.op(lambda e: e.transpose(trp[:, j, :], brs[:, j * 128:(j + 1) * 128], ident_b[:]), reads=[brs, ident_b], writes=[p2[1]])
                act.op(lambda e: e.copy(out=brT[:], in_=trp[:, 0:12, :]), reads=[p2[1]], writes=[brT])
                for i in range(3):
                    bo = p2[0]
                    for hf in range(2):
                        for kc in range(4):
                            pe.op(lambda e: e.matmul(bo[:, hf * 512:(hf + 1) * 512], brT[:, i * 4 + kc, :], wbr[:, i * 4 + kc, hf * 512:(hf + 1) * 512], start=(kc == 0), stop=(kc == 3)),
                                  reads=[brT, wbr], writes=[bo])
                    if i == 0:
                        dve.op(lambda e: e.tensor_tensor(out=acc[:], in0=bo[:], in1=gtb[:, 0:1024], op=ALU.mult), reads=[bo, gtb], writes=[acc])
                    else:
                        dve.op(lambda e: e.tensor_tensor(out=tmpm[:], in0=bo[:], in1=gtb[:, i * 1024:(i + 1) * 1024], op=ALU.mult), reads=[bo, gtb], writes=[tmpm])
                        pool.op(lambda e: e.tensor_tensor(out=acc[:], in0=acc[:], in1=tmpm[:], op=ALU.add), reads=[acc, tmpm], writes=[acc])
                act.op(lambda e: e.copy(out=accb[:], in_=acc[:]), reads=[acc], writes=[accb])
                for j in range(8):
                    pe.op(lambda e: e.transpose(trp[:, j, :], accb[:, j * 128:(j + 1) * 128], ident_b[:]), reads=[accb, ident_b], writes=[p2[1]])
                act.op(lambda e: e.copy(out=accT[:], in_=trp[:, 0:8, :]), reads=[p2[1]], writes=[accT])
                op_ = p2[0]
                for hf in range(2):
                    for kc in range(8):
                        pe.op(lambda e: e.matmul(op_[:, hf * 512:(hf + 1) * 512], accT[:, kc, :], wo[:, kc, hf * 512:(hf + 1) * 512], start=(kc == 0), stop=(kc == 7)), reads=[accT, wo], writes=[op_])
                dve.op(lambda e: e.tensor_tensor(out=tmpm[:], in0=op_[:], in1=g1t[:, si, :], op=ALU.mult), reads=[op_, g1t], writes=[tmpm])
                pool.op(lambda e: e.tensor_tensor(out=xr[:], in0=xr[:], in1=tmpm[:], op=ALU.add), reads=[xr, tmpm], writes=[xr])
                pool.dma(xres[r0:r0 + 128, :], xr[:], reads=[xr], writes=[D_x])
        fw.barrier()


    def phase_D(l, need_ctx=True):
        moe = (l % 2 == 1)
        idx = l // 2
        F_ = EFF if moe else FF
        NJ = F_ // 128
        NEXP = NE if moe else 1
        with ExitStack() as es:
            tabs = SB(es, "tabsD", [128, 3, D], F32)
            xb = SB(es, "xbD", [128, 4, D], F32)
            hn = [SB(es, "hn%d" % i, [128, D], F32) for i in range(2)]
            junk = SB(es, "junkD", [128, D], BF16)
            ssq = [SB(es, "ssq%d" % i, [128, 1], F32) for i in range(2)]
            h2T = SB(es, "h2T", [128, 8, 512], BF16)
            hT32 = SB(es, "hT32", [128, 8, 128], F32)
            aT = SB(es, "aT", [128, NJ, 512], BF16)
            slab = [SB(es, "slab%d" % i, [128, 8, 256], BF16) for i in range(3)]
            W2 = SB(es, "W2", [128, NJ, D], BF16)
            sg = [SB(es, "sg%d" % i, [128, 512], F32) for i in range(2)]
            tmp = [SB(es, "tmpD%d" % i, [128, 512], F32) for i in range(2)]
            pg = [PS(es, "pg%d" % i, [128, 512]) for i in range(2)]
            pu = [PS(es, "pu%d" % i, [128, 512]) for i in range(2)]
            po = [PS(es, "po%d" % i, [128, 512]) for i in range(2)]
            ptr = PS(es, "ptrD", [128, 512])
            plog = PS(es, "plog", [128, 512])
            if moe:
                yacc = SB(es, "yacc", [128, 4, D], F32)
                rw = SB(es, "rw", [128, 4, 8], F32)
                Wr = SB(es, "Wr", [128, 8, 8], F32)
                sp.dma(Wr[:], moe_router[idx].rearrange("(kc k) e -> k kc e", k=128), writes=[Wr])
                lgt = SB(es, "lgt", [128, 8], F32)
                m8 = SB(es, "m8", [128, 8], F32)
                nm = SB(es, "nm", [128, 1], F32)
                mk = SB(es, "mk", [128, 8], F32)
                ex = SB(es, "ex", [128, 8], F32)
                ssum = SB(es, "ssum", [128, 1], F32)
            blocks = ([(0, 256)] if need_ctx else []) + [(256 + 512 * j, 512) for j in range(16)]
            cur_set = None
            nslab = 0
            for (tok0, n) in blocks:
                si = 0 if tok0 < CTX else 1
                if si != cur_set:
                    for q in range(3):
                        sp.dma(tabs[:, q, :], modtab[l, si, 3 + q], writes=[tabs])
                    cur_set = si
                ntt = n // 128
                for tt in range(ntt):
                    r0 = tok0 + tt * 128
                    i = tt % 2
                    sp.dma(xb[:, tt, :], xres[r0:r0 + 128, :], writes=[xb])
                    act.op(lambda e: e.activation(out=junk[:], in_=xb[:, tt, :], func=AF.Square, accum_out=ssq[i][:]), reads=[xb], writes=[junk, ssq[i]])
                    rsqrt(ssq[i], ssq[i], 1.0 / D, EPS)
                    dve.op(lambda e: e.scalar_tensor_tensor(out=hn[i][:], in0=xb[:, tt, :], scalar=ssq[i][:, 0:1], in1=tabs[:, 1, :], op0=ALU.mult, op1=ALU.mult), reads=[xb, ssq[i], tabs], writes=[hn[i]])
                    pool.op(lambda e: e.tensor_tensor(out=hn[i][:], in0=hn[i][:], in1=tabs[:, 0, :], op=ALU.add), reads=[hn[i], tabs], writes=[hn[i]])
                    for hf in range(2):
                        for k4 in range(4):
                            kc = hf * 4 + k4
                            pe.op(lambda e: e.transpose(ptr[:, k4 * 128:(k4 + 1) * 128], hn[i][:, kc * 128:(kc + 1) * 128], ident_f[:]), reads=[hn[i], ident_f], writes=[ptr])
                        dve.op(lambda e: e.tensor_copy(out=h2T[:, hf * 4:(hf + 1) * 4, tt * 128:(tt + 1) * 128], in_=ptr[:].rearrange("p (a t) -> p a t", a=4)), reads=[ptr], writes=[h2T])
                        if moe:
                            act.op(lambda e: e.copy(out=hT32[:, hf * 4:(hf + 1) * 4, :], in_=ptr[:].rearrange("p (a t) -> p a t", a=4)), reads=[ptr], writes=[hT32])
                    if moe:
                        for kc in range(8):
                            pe.op(lambda e: e.matmul(plog[:, 0:8], hT32[:, kc, :], Wr[:, kc, :], start=(kc == 0), stop=(kc == 7)), reads=[hT32, Wr], writes=[plog])
                        dve.op(lambda e: e.tensor_copy(out=lgt[:], in_=plog[:, 0:8]), reads=[plog], writes=[lgt])
                        dve.op(lambda e: e.max(out=m8[:], in_=lgt[:]), reads=[lgt], writes=[m8])
                        dve.op(lambda e: e.tensor_scalar(out=nm[:], in0=m8[:, 0:1], scalar1=-1.0, scalar2=None, op0=ALU.mult), reads=[m8], writes=[nm])
                        dve.op(lambda e: e.tensor_scalar(out=mk[:], in0=lgt[:], scalar1=m8[:, 1:2], scalar2=None, op0=ALU.is_ge), reads=[lgt, m8], writes=[mk])
                        act.op(lambda e: e.activation(out=ex[:], in_=lgt[:], func=AF.Exp, bias=nm[:, 0:1], scale=1.0), reads=[lgt, nm], writes=[ex])
                        dve.op(lambda e: e.tensor_tensor(out=ex[:], in0=ex[:], in1=mk[:], op=ALU.mult), reads=[ex, mk], writes=[ex])
                        act.op(lambda e: e.activation(out=mk[:], in_=ex[:], func=AF.Identity, accum_out=ssum[:]), reads=[ex], writes=[mk, ssum])
                        dve.op(lambda e: e.reciprocal(out=ssum[:], in_=ssum[:]), reads=[ssum], writes=[ssum])
                        dve.op(lambda e: e.tensor_scalar(out=rw[:, tt, :], in0=ex[:], scalar1=ssum[:, 0:1], scalar2=None, op0=ALU.mult), reads=[ex, ssum], writes=[rw])
                for e_ in range(NEXP if not (moe and dstage in ('r', 'e1', 'e2', 'e4')) else {'r': 0, 'e1': 1, 'e2': 2, 'e4': 4}[dstage]):
                    slabs = m13s[idx, e_] if moe else f13s[idx]
                    w2src = m2b[idx, e_] if moe else f2b[idx]
                    for j in range(NJ):
                        sl = slab[nslab % 3]
                        nslab += 1
                        sp.dma(sl[:], slabs[j], writes=[sl])
                        if j == 2:
                            for ja in range(0, NJ, 7):
                                jb = min(ja + 7, NJ)
                                sp.dma(W2[:, ja:jb, :], w2src[ja * 128:jb * 128, :].rearrange("(j k) c -> k j c", k=128), writes=[W2])
                        g_, u_ = pg[j % 2], pu[j % 2]
                        for kc in range(8):
                            pe.op(lambda e: e.matmul(g_[:, 0:n], sl[:, kc, 0:128], h2T[:, kc, 0:n], start=(kc == 0), stop=(kc == 7)), reads=[sl, h2T], writes=[g_])
                        for kc in range(8):
                            pe.op(lambda e: e.matmul(u_[:, 0:n], sl[:, kc, 128:256], h2T[:, kc, 0:n], start=(kc == 0), stop=(kc == 7)), reads=[sl, h2T], writes=[u_])
                        s_ = sg[j % 2]
                        act.op(lambda e: e.activation(out=s_[:, 0:n], in_=g_[:, 0:n], func=AF.Silu), reads=[g_], writes=[s_])
                        dve.op(lambda e: e.tensor_tensor(out=aT[:, j, 0:n], in0=s_[:, 0:n], in1=u_[:, 0:n], op=ALU.mult), reads=[s_, u_], writes=[aT])
                    for tt in range(ntt):
                        for hf in range(2):
                            p_ = po[(tt * 2 + hf) % 2]
                            cs_ = slice(hf * 512, (hf + 1) * 512)
                            for j in range(NJ):
                                pe.op(lambda e: e.matmul(p_[:], aT[:, j, tt * 128:(tt + 1) * 128], W2[:, j, cs_], start=(j == 0), stop=(j == NJ - 1)), reads=[aT, W2], writes=[p_])
                            if moe:
                                if e_ == 0:
                                    dve.op(lambda e: e.tensor_scalar(out=yacc[:, tt, cs_], in0=p_[:], scalar1=rw[:, tt, 0:1], scalar2=None, op0=ALU.mult), reads=[p_, rw], writes=[yacc])
                                else:
                                    dve.op(lambda e: e.scalar_tensor_tensor(out=yacc[:, tt, cs_], in0=p_[:], scalar=rw[:, tt, e_:e_ + 1], in1=yacc[:, tt, cs_], op0=ALU.mult, op1=ALU.add),
                                           reads=[p_, rw, yacc], writes=[yacc])
                            else:
                                t_ = tmp[hf]
                                dve.op(lambda e: e.tensor_tensor(out=t_[:], in0=p_[:], in1=tabs[:, 2, cs_], op=ALU.mult), reads=[p_, tabs], writes=[t_])
                                pool.op(lambda e: e.tensor_tensor(out=xb[:, tt, cs_], in0=xb[:, tt, cs_], in1=t_[:], op=ALU.add), reads=[xb, t_], writes=[xb])
                    if moe:
                        fw.barrier()
                for tt in range(ntt):
                    r0 = tok0 + tt * 128
                    if moe and dstage != 'r':
                        dve.op(lambda e: e.tensor_tensor(out=yacc[:, tt, :], in0=yacc[:, tt, :], in1=tabs[:, 2, :], op=ALU.mult), reads=[yacc, tabs], writes=[yacc])
                        pool.op(lambda e: e.tensor_tensor(out=xb[:, tt, :], in0=xb[:, tt, :], in1=yacc[:, tt, :], op=ALU.add), reads=[xb, yacc], writes=[xb])
                    pool.dma(xres[r0:r0 + 128, :], xb[:, tt, :], reads=[xb], writes=[D_x])
        fw.barrier()

    def phase_final():
        with ExitStack() as es:
            gt = SB(es, "gtF", [128, D], F32)
            sp.dma(gt[:], fin_g[0:1, :].partition_broadcast(128), writes=[gt])
            xt = [SB(es, "xtF%d" % i, [128, D], F32) for i in range(3)]
            ot = [SB(es, "otF%d" % i, [128, D], F32) for i in range(2)]
            junk = SB(es, "junkF", [128, D], BF16)
            ssq = [SB(es, "ssqF%d" % i, [128, 1], F32) for i in range(2)]
            for c in range(2, NCH):
                x_ = xt[c % 3]
                i = c % 2
                sp.dma(x_[:], xres[c * 128:(c + 1) * 128, :], writes=[x_])
                act.op(lambda e: e.activation(out=junk[:], in_=x_[:], func=AF.Square, accum_out=ssq[i][:]), reads=[x_], writes=[junk, ssq[i]])
                rsqrt(ssq[i], ssq[i], 1.0 / D, EPS)
                dve.op(lambda e: e.scalar_tensor_tensor(out=ot[i][:], in0=x_[:], scalar=ssq[i][:, 0:1], in1=gt[:], op0=ALU.mult, op1=ALU.mult), reads=[x_, ssq[i], gt], writes=[ot[i]])
                pool.dma(yout[(c - 2) * 128:(c - 1) * 128, :], ot[i][:], reads=[ot[i]], writes=[D_x])
        fw.barrier()

    K.nc = nc
    K.fw = fw
    layers = list(range(depth))
    phase_cast(layers)
    phase_mod(layers)
    for l in layers:
        last = (l == DEPTH - 1)
        phase_A0(l)
        if upto == 'A0' and l == upto_layer:
            break
        phase_A1(l)
        if upto == 'A1' and l == upto_layer:
            break
        phase_B1(l)
        phase_B2(l)
        if upto == 'B2' and l == upto_layer:
            break
        phase_C(l, need_ctx=not last)
        if upto == 'C' and l == upto_layer:
            break
        phase_D(l, need_ctx=not last)
    if upto is None:
        phase_final()
    fw.barrier()
    es_all.close()
    return nc


def make_in_map(I, b, depth=DEPTH):
    cs = host_consts()
    m = {}
    m["xin"] = np.ascontiguousarray(np.concatenate([I["ctx"][b], I["x"][b]], axis=0))
    m["cvec"] = np.ascontiguousarray(np.stack([I["c_ctx"], I["c"][b]], axis=0))
    for k in ("ada_w", "ada_b", "norm1_g", "norm2_g", "w_in", "ssd_conv_w", "ssd_conv_b", "ssd_d", "ssd_norm_g", "pool_w",
              "pool_scale", "w_out", "ffn_w13", "ffn_w2", "moe_router", "moe_w13", "moe_w2"):
        m[k] = np.ascontiguousarray(I[k])
    m["ssd_dt_bias"] = np.ascontiguousarray(I["ssd_dt_bias"].reshape(DEPTH, 16))
    m["ssd_a_log"] = np.ascontiguousarray(I["ssd_a_log"].reshape(DEPTH, 16))
    m["ret_decay_logit"] = np.ascontiguousarray(I["ret_decay_logit"].reshape(DEPTH, 16))
    m["w_branch"] = np.ascontiguousarray(I["w_branch"].reshape(DEPTH, 1536, D))
    m["final_norm_g"] = np.ascontiguousarray(I["final_norm_g"].reshape(1, D))
    if depth // 2 == 0:
        m["moe_w13"] = np.zeros((1, 1, 128, 128), np.float32)
        m["moe_w2"] = np.zeros((1, 1, 128, 128), np.float32)
    for k, v in cs.items():
        m["c_" + k] = np.ascontiguousarray(v)
    return m


def kernel(**inputs):
    nc = build()
    in_maps = [make_in_map(inputs, b) for b in range(4)]
    res = run_bass_kernel_spmd(nc, in_maps, core_ids=[0, 1, 2, 3])
    return np.stack([np.asarray(res.results[b]["yout"]) for b in range(4)], axis=0).astype(np.float32)
```
